# Optimizing a Trainium2 kernel written in Bass

```python
import math
import jax, jax.numpy as jnp
from jax import lax
import numpy as np

D_MODEL = 1024
BATCH = 4
SEQ = 8192
DEPTH = 2

CHUNK = 64
Q_BLOCK = 128
PLE_DIM = 256
RMS_EPS = 1e-6

SB_HEADS = 8
SB_HEAD_DIM = 64
SB_WIDTH = SB_HEADS * SB_HEAD_DIM

DA_HEADS = 4
DA_HEAD_DIM = 64
DA_V_DIM = 2 * DA_HEAD_DIM
DA_QK_WIDTH = DA_HEADS * 2 * DA_HEAD_DIM
DA_WIDTH = DA_HEADS * DA_V_DIM

ROPE_THETA = 500000.0
ROPE_DIM = DA_HEAD_DIM // 4

N_GROUPS = 4
EXPERTS_PER_GROUP = 8
N_EXPERTS = N_GROUPS * EXPERTS_PER_GROUP
TOP_K = 2
EXPERT_HIDDEN = 256
MOE_BLOCK = 256

IN_SIZES = (SB_WIDTH, SB_WIDTH, SB_WIDTH, DA_QK_WIDTH, DA_QK_WIDTH, DA_WIDTH, D_MODEL, D_MODEL)
IN_WIDTH = sum(IN_SIZES)
IN_OFFSETS = tuple(int(o) for o in np.cumsum(IN_SIZES)[:-1])

kernel_name = "hybrid_stickbreak_diffattn_hmoe_block"


def rmsnorm(x, g):
    xf = x.astype(jnp.float32)
    y = xf * lax.rsqrt(jnp.mean(xf * xf, axis=-1, keepdims=True) + RMS_EPS)
    return (y * g.astype(jnp.float32)).astype(x.dtype)


def apply_partial_rope(t, cos, sin):
    half = ROPE_DIM // 2
    r1 = t[..., :half]
    r2 = t[..., half:ROPE_DIM]
    rot = jnp.concatenate([r1 * cos - r2 * sin, r2 * cos + r1 * sin], axis=-1)
    return jnp.concatenate([rot, t[..., ROPE_DIM:]], axis=-1)


def stick_breaking_attention(q, k, v):
    B, H, S, d = q.shape
    scale = d ** -0.5
    key_idx = jnp.arange(S)

    def block(j):
        start = j * Q_BLOCK
        qb = lax.dynamic_slice_in_dim(q, start, Q_BLOCK, axis=2)
        z = jnp.einsum('bhqd,bhkd->bhqk', qb, k, preferred_element_type=jnp.float32) * scale
        q_idx = start + jnp.arange(Q_BLOCK)
        strict = key_idx[None, :] < q_idx[:, None]
        log_stay = jnp.where(strict, jax.nn.log_sigmoid(-z), 0.0)
        between = lax.cumsum(log_stay, axis=3, reverse=True) - log_stay
        w = jnp.where(strict, jnp.exp(jax.nn.log_sigmoid(z) + between), 0.0)
        return jnp.einsum('bhqk,bhkd->bhqd', w.astype(v.dtype), v)

    out = lax.map(block, jnp.arange(S // Q_BLOCK))
    return out.transpose(1, 0, 3, 2, 4).reshape(B, S, H, d)


def differential_attention(q, k, v, lam):
    B, H, _, S, d = q.shape
    dv = v.shape[-1]
    scale = d ** -0.5
    key_chunk = jnp.arange(S) // CHUNK
    neg = jnp.finfo(jnp.float32).min

    def block(j):
        start = j * Q_BLOCK
        qb = lax.dynamic_slice_in_dim(q, start, Q_BLOCK, axis=3)
        s = jnp.einsum('bhmqd,bhmkd->bhmqk', qb, k, preferred_element_type=jnp.float32) * scale
        q_chunk = (start + jnp.arange(Q_BLOCK)) // CHUNK
        allowed = key_chunk[None, :] <= q_chunk[:, None]
        a = jax.nn.softmax(jnp.where(allowed, s, neg), axis=-1)
        a = a[:, :, 0] - lam * a[:, :, 1]
        return jnp.einsum('bhqk,bhkd->bhqd', a.astype(v.dtype), v)

    out = lax.map(block, jnp.arange(S // Q_BLOCK))
    return out.transpose(1, 0, 3, 2, 4).reshape(B, S, H, dv)


def hierarchical_moe(h, w_rg, b_rg, w_re, b_re, w_gate, w_up, w_down):
    B, S, D = h.shape
    N = B * S
    t = h.reshape(N, D)
    rows = jnp.arange(N)
    g_logits = (t @ w_rg).astype(jnp.float32) + b_rg.astype(jnp.float32)
    g_prob = jax.nn.softmax(g_logits, axis=-1)
    g_sel = jnp.argmax(g_logits, axis=-1)
    g_p = g_prob[rows, g_sel]
    e_logits = ((t @ w_re).astype(jnp.float32) + b_re.astype(jnp.float32)).reshape(N, N_GROUPS, EXPERTS_PER_GROUP)
    e_prob = jax.nn.softmax(e_logits[rows, g_sel], axis=-1)
    top_p, top_i = lax.top_k(e_prob, TOP_K)
    gates = g_p[:, None] * top_p / jnp.sum(top_p, axis=-1, keepdims=True)

    expert_id = (g_sel[:, None] * EXPERTS_PER_GROUP + top_i).reshape(-1)
    token_id = jnp.repeat(rows, TOP_K)
    gate_flat = gates.reshape(-1)
    A = N * TOP_K

    order = jnp.argsort(expert_id)
    se, st, sg = expert_id[order], token_id[order], gate_flat[order]
    counts = jnp.zeros((N_EXPERTS,), jnp.int32).at[expert_id].add(1)
    pcounts = (counts + MOE_BLOCK - 1) // MOE_BLOCK * MOE_BLOCK
    starts = jnp.cumsum(counts) - counts
    pends = jnp.cumsum(pcounts)
    pstarts = pends - pcounts
    dest = pstarts[se] + jnp.arange(A) - starts[se]
    P = ((A + MOE_BLOCK - 1) // MOE_BLOCK) * MOE_BLOCK + N_EXPERTS * MOE_BLOCK
    n_blk = P // MOE_BLOCK
    slot_tok = jnp.full((P,), N, jnp.int32).at[dest].set(st)
    slot_gate = jnp.zeros((P,), jnp.float32).at[dest].set(sg)
    blk_e = jnp.minimum(jnp.searchsorted(pends, jnp.arange(n_blk) * MOE_BLOCK, side='right'), N_EXPERTS - 1)
    xs = jnp.concatenate([t, jnp.zeros((1, D), t.dtype)], axis=0)[slot_tok].reshape(n_blk, MOE_BLOCK, D)

    def expert_block(args):
        xb, e = args
        hid = jax.nn.silu(xb @ w_gate[e]) * (xb @ w_up[e])
        return hid @ w_down[e]

    ys = lax.map(expert_block, (xs, blk_e)).reshape(P, D)
    ys = ys * slot_gate[:, None].astype(ys.dtype)
    out = jax.ops.segment_sum(ys, slot_tok, num_segments=N + 1)[:N]
    return out.reshape(B, S, D)


def setup_inputs(seed: int = 0) -> dict:
    key = jax.random.key(seed)
    ks = jax.random.split(key, 26)
    D = D_MODEL

    def nrm(k, shape, scale):
        return jax.random.normal(k, shape, jnp.float32) * scale

    def gain(k, shape):
        return 1.0 + 0.02 * jax.random.normal(k, shape, jnp.float32)

    offset = jax.random.randint(ks[2], (BATCH, 1), 0, 4096, dtype=jnp.int32)
    positions = (offset + jnp.arange(SEQ, dtype=jnp.int32)[None, :]).astype(jnp.int32)
    return {
        "x": nrm(ks[0], (BATCH, SEQ, D), 1.0),
        "p": nrm(ks[1], (DEPTH, BATCH, SEQ, PLE_DIM), 1.0),
        "positions": positions,
        "g_mix": gain(ks[3], (DEPTH, D)),
        "w_in": nrm(ks[4], (DEPTH, D, IN_WIDTH), D ** -0.5),
        "lam_q1": nrm(ks[5], (DEPTH, DA_HEAD_DIM), 0.1),
        "lam_k1": nrm(ks[6], (DEPTH, DA_HEAD_DIM), 0.1),
        "lam_q2": nrm(ks[7], (DEPTH, DA_HEAD_DIM), 0.1),
        "lam_k2": nrm(ks[8], (DEPTH, DA_HEAD_DIM), 0.1),
        "g_subln": gain(ks[9], (DEPTH, DA_V_DIM)),
        "w_br_a": nrm(ks[10], (DEPTH, SB_WIDTH, D), SB_WIDTH ** -0.5),
        "w_br_b": nrm(ks[11], (DEPTH, DA_WIDTH, D), DA_WIDTH ** -0.5),
        "w_o": nrm(ks[12], (DEPTH, D, D), D ** -0.5),
        "g_ffn": gain(ks[13], (DEPTH, D)),
        "w_router_group": nrm(ks[14], (DEPTH, D, N_GROUPS), D ** -0.5),
        "b_router_group": nrm(ks[15], (DEPTH, N_GROUPS), 0.01),
        "w_router_expert": nrm(ks[16], (DEPTH, D, N_EXPERTS), D ** -0.5),
        "b_router_expert": nrm(ks[17], (DEPTH, N_EXPERTS), 0.01),
        "w_exp_gate": nrm(ks[18], (DEPTH, N_EXPERTS, D, EXPERT_HIDDEN), D ** -0.5),
        "w_exp_up": nrm(ks[19], (DEPTH, N_EXPERTS, D, EXPERT_HIDDEN), D ** -0.5),
        "w_exp_down": nrm(ks[20], (DEPTH, N_EXPERTS, EXPERT_HIDDEN, D), EXPERT_HIDDEN ** -0.5),
        "g_ple": gain(ks[21], (DEPTH, D)),
        "w_ple": nrm(ks[22], (DEPTH, PLE_DIM, D), PLE_DIM ** -0.5),
        "w_ple_gate": nrm(ks[23], (DEPTH, D, D), D ** -0.5),
        "g_final": gain(ks[24], (D,)),
    }


def reference(x, p, positions, g_mix, w_in, lam_q1, lam_k1, lam_q2, lam_k2, g_subln,
              w_br_a, w_br_b, w_o, g_ffn, w_router_group, b_router_group,
              w_router_expert, b_router_expert, w_exp_gate, w_exp_up, w_exp_down,
              g_ple, w_ple, w_ple_gate, g_final):
    B, S, D = x.shape
    inv_freq = ROPE_THETA ** (-jnp.arange(0, ROPE_DIM, 2, dtype=jnp.float32) / ROPE_DIM)
    ang = positions.astype(jnp.float32)[..., None] * inv_freq
    cos = jnp.cos(ang)[:, :, None, None, :].astype(x.dtype)
    sin = jnp.sin(ang)[:, :, None, None, :].astype(x.dtype)

    for i in range(DEPTH):
        lam_init = 0.8 - 0.6 * math.exp(-0.3 * i)
        h = rmsnorm(x, g_mix[i])
        u = h @ w_in[i]
        sb_q, sb_k, sb_v, da_q, da_k, da_v, gate_a, gate_b = jnp.split(u, IN_OFFSETS, axis=-1)

        to_heads = lambda t: t.reshape(B, S, SB_HEADS, SB_HEAD_DIM).transpose(0, 2, 1, 3)
        y_a = stick_breaking_attention(to_heads(sb_q), to_heads(sb_k), to_heads(sb_v))
        y_a = y_a.reshape(B, S, SB_WIDTH) @ w_br_a[i]

        dq = apply_partial_rope(da_q.reshape(B, S, DA_HEADS, 2, DA_HEAD_DIM), cos, sin)
        dk = apply_partial_rope(da_k.reshape(B, S, DA_HEADS, 2, DA_HEAD_DIM), cos, sin)
        dq = dq.transpose(0, 2, 3, 1, 4)
        dk = dk.transpose(0, 2, 3, 1, 4)
        dv = da_v.reshape(B, S, DA_HEADS, DA_V_DIM).transpose(0, 2, 1, 3)
        lam = (jnp.exp(jnp.sum(lam_q1[i].astype(jnp.float32) * lam_k1[i].astype(jnp.float32)))
               - jnp.exp(jnp.sum(lam_q2[i].astype(jnp.float32) * lam_k2[i].astype(jnp.float32)))
               + lam_init)
        y_b = differential_attention(dq, dk, dv, lam)
        y_b = rmsnorm(y_b, g_subln[i]) * (1.0 - lam_init)
        y_b = y_b.reshape(B, S, DA_WIDTH) @ w_br_b[i]

        merged = jax.nn.sigmoid(gate_a) * y_a + jax.nn.sigmoid(gate_b) * y_b
        x = x + merged @ w_o[i]

        x = x + hierarchical_moe(rmsnorm(x, g_ffn[i]), w_router_group[i], b_router_group[i],
                                 w_router_expert[i], b_router_expert[i],
                                 w_exp_gate[i], w_exp_up[i], w_exp_down[i])

        ple_gate = jax.nn.sigmoid(rmsnorm(x, g_ple[i]) @ w_ple_gate[i])
        x = x + (p[i] @ w_ple[i]) * ple_gate

    return rmsnorm(x, g_final)
```

```python
import math
import numpy as np
import concourse.bass as bass
import concourse.mybir as mybir
from concourse.bass_utils import run_bass_kernel_spmd

F32, BF16, I32 = mybir.dt.float32, mybir.dt.bfloat16, mybir.dt.int32
AF = mybir.ActivationFunctionType
ALU = mybir.AluOpType

D = 1024
NE = 32
EH = 256
EPS = 1e-6
TWO_PI = 2.0 * math.pi
N_CORES = 8


class _Op:
    __slots__ = ("eng", "fn", "deps", "needs_inc", "cnt", "phase", "slot", "dmacnt", "waits")


class Prog:
    CE = ("pe", "act", "dve", "pool")
    ALL = ("pe", "act", "dve", "pool", "sp")

    def __init__(self):
        self.ops = {e: [] for e in self.ALL}
        self.lw = {}
        self.rd = {}
        self.phase = 0
        self.slotcnt = {}
        self.pending_dma = []
        self.slotmap = {}
        self.epoch = {e: 0 for e in self.ALL}
        self.opcount = {e: 0 for e in self.ALL}

    def add(self, eng, fn, reads=(), writes=(), slot=None):
        if slot is not None:
            if slot not in self.slotmap:
                self.slotmap[slot] = "g%d" % len(self.slotmap)
            slot = self.slotmap[slot]
        op = _Op()
        op.eng, op.fn, op.phase, op.slot = eng, fn, self.epoch[eng], slot
        self.opcount[eng] += 1
        op.needs_inc = False
        op.cnt = 0
        op.dmacnt = 0
        deps = []
        raw = set()
        for r in reads:
            w = self.lw.get(r)
            if w is not None:
                deps.append(w)
                raw.add(id(w))
        for r in writes:
            w = self.lw.get(r)
            if w is not None:
                deps.append(w)
            rr = self.rd.get(r)
            if rr:
                deps.extend(rr[0].values())
                deps.extend(rr[1])
        is_dma = slot is not None
        dd = []
        seen = set()
        for d in deps:
            if id(d) in seen:
                continue
            seen.add(id(d))
            if d.slot is not None:
                dd.append(d)
            elif d.eng != eng or is_dma or (eng != "pe" and id(d) in raw):
                d.needs_inc = True
                dd.append(d)
        op.deps = dd
        for r in reads:
            rr = self.rd.get(r)
            if rr is None:
                rr = self.rd[r] = ({}, [])
            if is_dma:
                rr[1].append(op)
            else:
                rr[0][eng] = op
        for r in writes:
            self.lw[r] = op
            self.rd[r] = ({}, [])
        if is_dma:
            self.slotcnt[slot] = self.slotcnt.get(slot, 0) + 1
            op.dmacnt = 16 * self.slotcnt[slot]
            self.pending_dma.append(op)
        self.ops[eng].append(op)
        return op

    def barrier(self):
        lasts = []
        for e in self.CE:
            for o in reversed(self.ops[e]):
                if o.slot is None:
                    if o.fn is not None:
                        lasts.append(o)
                    break
        pend = self.pending_dma
        self.pending_dma = []
        for e in self.ALL:
            op = _Op()
            op.eng, op.fn, op.phase, op.slot = e, None, self.epoch[e], None
            op.needs_inc = False
            op.cnt = 0
            op.dmacnt = 0
            op.deps = []
            for d in lasts:
                if d.eng != e and d.fn is not None:
                    d.needs_inc = True
                    op.deps.append(d)
            op.deps.extend(pend)
            self.ops[e].append(op)
        self.phase += 1
        self.slotmap = {}
        for e in self.ALL:
            if self.opcount[e] > 12000:
                self.epoch[e] += 1
                self.opcount[e] = 0

    def finalize(self):
        for e in self.ALL:
            cnt = {}
            for op in self.ops[e]:
                if op.slot is not None or op.fn is None:
                    op.needs_inc = False
                if op.needs_inc:
                    cnt[op.phase] = cnt.get(op.phase, 0) + 1
                    op.cnt = cnt[op.phase]
                    assert op.cnt < 30000, "semaphore count overflow"
        for s, c in self.slotcnt.items():
            assert 16 * c < 32000, f"dma slot {s} overflow {c}"
        for e in self.ALL:
            waited = {}
            for op in self.ops[e]:
                need = {}
                for d in op.deps:
                    if d.slot is not None:
                        k, v = ("dma", d.slot), d.dmacnt
                    else:
                        k, v = (d.eng, d.phase), d.cnt
                    if waited.get(k, 0) >= v:
                        continue
                    if need.get(k, 0) < v:
                        need[k] = v
                for k, v in need.items():
                    waited[k] = v
                op.waits = list(need.items())


def _consts_host(S):
    c = {}
    c["ident"] = np.eye(128, dtype=np.float32)
    j = np.arange(128)[:, None]
    s = np.arange(128)[None, :]
    c["trineg"] = -(j >= s).astype(np.float32)
    c["ones"] = np.ones((128, 128), np.float32)
    t = np.arange(512)[None, :]
    sb = [((128 * jj + np.arange(128)[:, None]) < t).astype(np.float32) for jj in range(4)]
    da = [(((128 * jj + np.arange(128)[:, None]) // 64) <= (t // 64)).astype(np.float32) for jj in range(4)]
    c["msb"] = np.concatenate(sb, axis=1)
    c["mda"] = np.concatenate(da, axis=1)
    inv_freq = (500000.0 ** (-np.arange(0, 16, 2, dtype=np.float32) / 16.0)).astype(np.float32)
    f = np.zeros((128,), np.float32)
    sg = np.zeros((128,), np.float32)
    for p in range(128):
        i = p % 64
        if i < 8:
            f[p] = inv_freq[i]
            sg[p] = -1.0
        elif i < 16:
            f[p] = inv_freq[i - 8]
            sg[p] = 1.0
    ns = np.zeros((128,), np.float32)
    ns[1] = -1.0
    c["ropef"] = np.stack([f, sg, ns], axis=1)
    return np.concatenate([c["ident"], c["trineg"], c["ones"], c["msb"], c["mda"], c["ropef"]], axis=1).astype(np.float32)


C_IDENT, C_TRI, C_ONES, C_MSB, C_MDA, C_ROPE = 0, 128, 256, 384, 384 + 2048, 384 + 4096
C_TOTAL = 384 + 4096 + 3


def build_program(S, L, lam_inits, debug=False):
    import os
    debug = debug or os.environ.get("KDEBUG") == "1"
    NT = S // 128
    NG = S // 512
    nc = bass.Bass("TRN2", target_bir_lowering=False)
    P = Prog()

    def din(name, shape, dt=F32):
        return nc.dram_tensor(name, list(shape), dt, kind="ExternalInput").ap()

    def dscr(name, shape, dt):
        kind = "ExternalOutput" if debug else "Internal"
        return nc.dram_tensor(name, list(shape), dt, kind=kind).ap()

    x_in = din("x", [S, D])
    p_in = din("p", [L, S, 256])
    pos_in = din("pos", [1, S], I32)
    consts_in = din("consts", [128, C_TOTAL])
    gcols_in = din("gcols", [L, 128, 24])
    w_in_in = din("w_in", [L, D, 6144])
    lam_in = din("lam", [L, 1, 256])
    gsub_in = din("gsub", [L, 128, 1])
    w_a_in = din("w_a", [L, 512, D])
    w_b_in = din("w_b", [L, 512, D])
    w_o_in = din("w_o", [L, D, D])
    w_r_in = din("w_r", [L, D, 36])
    b_r_in = din("b_r", [L, 1, 36])
    w_eg_in = din("w_eg", [L, NE, D, EH])
    w_eu_in = din("w_eu", [L, NE, D, EH])
    w_ed_in = din("w_ed", [L, NE, EH, D])
    w_ple_in = din("w_ple", [L, 256, D])
    w_pg_in = din("w_pg", [L, D, D])
    gfin_in = din("gfin", [1, D])
    out_d = nc.dram_tensor("out", [S, D], F32, kind="ExternalOutput").ap()

    xs = dscr("xs", [S, D], F32)
    qkT = dscr("qkT", [16, 128, S], BF16)
    sgT = dscr("sgT", [16, 128, S], BF16)
    vsc = dscr("vsc", [S, 1024], BF16)
    atT = dscr("atT", [8, 128, S], BF16)
    h2T = dscr("h2T", [8, 128, S], BF16)
    ropeT = dscr("ropeT", [2, 128, S], F32)

    ARENA_W = 52400
    import contextlib
    es = contextlib.ExitStack()
    arena = es.enter_context(nc.sbuf_tensor("arena", [128, ARENA_W], F32))
    psum = es.enter_context(nc.psum_tensor("psum", [128, 4096], F32))

    def bank(b, n=1):
        return psum[:, b * 512:(b + n) * 512]

    class Arena:
        def __init__(self):
            self.off = 0
            self.mark = 0

        def alloc(self, nelem, dt=F32, parts=128):
            nb = nelem * (4 if dt in (F32, I32) else 2)
            w = (nb + 3) // 4
            assert self.off + w <= ARENA_W, f"arena overflow {self.off + w}"
            a = arena[0:parts, self.off:self.off + w]
            self.off += w
            if dt != F32:
                a = a.bitcast(dt)
            return a

        def set_mark(self):
            self.mark = self.off

        def reset(self):
            self.off = self.mark

    A = Arena()

    def pe(fn, r, w):
        return P.add("pe", fn, r, w)

    def act(fn, r, w):
        return P.add("act", fn, r, w)

    def dve(fn, r, w):
        return P.add("dve", fn, r, w)

    def pool(fn, r, w):
        return P.add("pool", fn, r, w)

    def dma(out, in_, r, w, slot, q="sp"):
        return P.add(q, lambda e: e.dma_start(out=out, in_=in_), r, w, slot=slot)

    def mm(out, lhsT, rhs, start, stop, r, w):
        return pe(lambda e: e.matmul(out, lhsT, rhs, start=start, stop=stop), r, w)

    ident_f = A.alloc(128)
    ropef = A.alloc(3)
    ident_b = A.alloc(128, BF16)
    tri_b = A.alloc(128, BF16)
    ones_b = A.alloc(128, BF16)
    msb_b = A.alloc(2048, BF16)
    mda_b = A.alloc(2048, BF16)
    tri2 = A.alloc(2, BF16)
    ones2 = A.alloc(128, BF16, parts=2)
    gcols = A.alloc(24 * L)
    gsub = A.alloc(L)
    lamt = A.alloc(256 * L)
    lamw = A.alloc(8)
    neglam = A.alloc(L)
    gfin_bc = A.alloc(D)
    brt = A.alloc(36 * L)
    Gt = A.alloc(NT * 32)
    carry = [A.alloc(512, BF16, parts=2) for _ in range(2)]
    hi2 = A.alloc(512, BF16, parts=2)
    smalls = A.alloc(64)
    A.set_mark()
    cst_f = A.alloc(C_TOTAL)
    negsel = ropef[0:2, 2:3]

    dma(cst_f, consts_in[:, :], [], ["cst_f"], "c0")
    for l in range(L):
        dma(gcols[:, 24 * l:24 * l + 24], gcols_in[l], [], [("gcols", l)], f"c1{l}")
        dma(gsub[:, l:l + 1], gsub_in[l], [], [("gsub", l)], f"c2{l}")
        dma(lamt[:, 256 * l:256 * l + 256], lam_in[l].partition_broadcast(128), [], [("lamt", l)], f"c3{l}")
        dma(brt[:, 36 * l:36 * l + 36], b_r_in[l].partition_broadcast(128), [], [("brt", l)], f"c4{l}")
    dma(gfin_bc, gfin_in.partition_broadcast(128), [], ["gfin"], "c5")
    dve(lambda e: e.tensor_copy(out=ident_f, in_=cst_f[:, C_IDENT:C_IDENT + 128]), ["cst_f"], ["ident_f"])
    dve(lambda e: e.tensor_copy(out=ropef, in_=cst_f[:, C_ROPE:C_ROPE + 3]), ["cst_f"], ["ropef"])
    dve(lambda e: e.tensor_copy(out=ident_b, in_=cst_f[:, C_IDENT:C_IDENT + 128]), ["cst_f"], ["ident_b"])
    dve(lambda e: e.tensor_copy(out=tri_b, in_=cst_f[:, C_TRI:C_TRI + 128]), ["cst_f"], ["tri_b"])
    dve(lambda e: e.tensor_copy(out=ones_b, in_=cst_f[:, C_ONES:C_ONES + 128]), ["cst_f"], ["ones_b"])
    dve(lambda e: e.tensor_copy(out=msb_b, in_=cst_f[:, C_MSB:C_MSB + 2048]), ["cst_f"], ["msb_b"])
    dve(lambda e: e.tensor_copy(out=mda_b, in_=cst_f[:, C_MDA:C_MDA + 2048]), ["cst_f"], ["mda_b"])
    dve(lambda e: e.tensor_scalar(out=tri2, in0=cst_f[:, C_ONES:C_ONES + 2], scalar1=-1.0, scalar2=None, op0=ALU.mult), ["cst_f"], ["tri2"])
    dve(lambda e: e.tensor_copy(out=ones2, in_=cst_f[0:2, C_ONES:C_ONES + 128]), ["cst_f"], ["ones2"])
    for l in range(L):
        lt = lamt[:, 256 * l:256 * l + 256]
        junk = smalls[:, 0:64]
        dve(lambda e, lt=lt: e.tensor_tensor(out=junk, in0=lt[:, 0:64], in1=lt[:, 64:128], op=ALU.mult), [("lamt", l)], ["junk_s"])
        dve(lambda e: e.reduce_sum(out=lamw[:, 0:1], in_=junk, axis=mybir.AxisListType.X), ["junk_s"], ["lamw"])
        dve(lambda e, lt=lt: e.tensor_tensor(out=junk, in0=lt[:, 128:192], in1=lt[:, 192:256], op=ALU.mult), [("lamt", l)], ["junk_s"])
        dve(lambda e: e.reduce_sum(out=lamw[:, 1:2], in_=junk, axis=mybir.AxisListType.X), ["junk_s"], ["lamw"])
        act(lambda e: e.activation(out=lamw[:, 2:4], in_=lamw[:, 0:2], func=AF.Exp), ["lamw"], ["lamw2"])
        dve(lambda e: e.tensor_tensor(out=lamw[:, 4:5], in0=lamw[:, 3:4], in1=lamw[:, 2:3], op=ALU.subtract), ["lamw2"], ["lamw3"])
        dve(lambda e, l=l: e.tensor_scalar(out=neglam[:, l:l + 1], in0=lamw[:, 4:5], scalar1=-float(lam_inits[l]), scalar2=None, op0=ALU.add),
            ["lamw3"], [("neglam", l)])

    def phase_rope():
        A.reset()
        posi = A.alloc(S, I32)
        ang = A.alloc(S)
        tq = A.alloc(S)
        ki = A.alloc(S, I32)
        rr = A.alloc(S)
        dma(posi, pos_in.partition_broadcast(128), [], ["posi"], "c0")
        dve(lambda e: e.tensor_copy(out=ang, in_=posi), ["posi"], ["ang"])
        dve(lambda e: e.tensor_scalar(out=ang, in0=ang, scalar1=ropef[:, 0:1], scalar2=None, op0=ALU.mult), ["ang", "ropef"], ["ang"])
        for which in range(2):
            shift = math.pi / 2 if which == 0 else 0.0
            dve(lambda e, shift=shift: e.tensor_scalar(out=tq, in0=ang, scalar1=shift, scalar2=1.0 / TWO_PI, op0=ALU.add, op1=ALU.mult), ["ang"], ["tq"])
            dve(lambda e: e.tensor_copy(out=ki, in_=tq), ["tq"], ["ki"])
            dve(lambda e: e.tensor_copy(out=tq, in_=ki), ["ki"], ["tq"])
            dve(lambda e: e.scalar_tensor_tensor(out=rr, in0=tq, scalar=-TWO_PI, in1=ang, op0=ALU.mult, op1=ALU.add), ["tq", "ang"], ["rr"])
            if which == 0:
                dve(lambda e: e.tensor_scalar(out=rr, in0=rr, scalar1=math.pi / 2, scalar2=None, op0=ALU.add), ["rr"], ["rr"])
            dve(lambda e: e.tensor_scalar(out=rr, in0=rr, scalar1=math.pi, scalar2=-math.pi, op0=ALU.min, op1=ALU.max), ["rr"], ["rr"])
            if which == 0:
                act(lambda e: e.activation(out=tq, in_=rr, func=AF.Sin), ["rr"], ["tq"])
            else:
                act(lambda e: e.activation(out=tq, in_=rr, func=AF.Sin), ["rr"], ["tq"])
                dve(lambda e: e.tensor_scalar(out=tq, in0=tq, scalar1=ropef[:, 1:2], scalar2=None, op0=ALU.mult), ["tq", "ropef"], ["tq"])
            dma(ropeT[which], tq, ["tq"], [("ropeT", which)], "st0")
        P.barrier()

    def load_cast(dst_b, src_dram, nchunk, ncol, stage, skeys, dkey, slot, gcol=None, gkey=None, eng="pool"):
        st3 = stage.rearrange("p (c n) -> p c n", c=nchunk)
        dma(st3, src_dram.rearrange("(c p) n -> p c n", p=128), [], list(skeys), slot)
        add = pool if eng == "pool" else (dve if eng == "dve" else act)
        if gcol is None:
            add(lambda e: e.tensor_copy(out=dst_b, in_=st3), list(skeys), [dkey])
        else:
            add(lambda e: e.tensor_tensor(out=dst_b, in0=st3, in1=gcol.unsqueeze(2).broadcast_to([128, nchunk, ncol]), op=ALU.mult),
                list(skeys) + [gkey], [dkey])

    def rms_rstd(xt, ss, rstd, junk, keyx, keyr):
        act(lambda e: e.activation(out=junk, in_=xt, func=AF.Square, accum_out=ss), [keyx], ["junk_n", keyr + "_ss"])
        dve(lambda e: e.tensor_scalar(out=ss, in0=ss, scalar1=1.0 / D, scalar2=EPS, op0=ALU.mult, op1=ALU.add), [keyr + "_ss"], [keyr + "_ss2"])
        act(lambda e: e.activation(out=ss, in_=ss, func=AF.Ln), [keyr + "_ss2"], [keyr + "_ss3"])
        act(lambda e: e.activation(out=rstd, in_=ss, func=AF.Exp, scale=-0.5), [keyr + "_ss3"], [keyr])

    def phase_inproj(l):
        A.reset()
        xsrc = x_in if l == 0 else xs
        GP = 256
        NGP = S // GP
        TPG = GP // 128
        Wb = A.alloc(8 * 6144, BF16).rearrange("p (c n) -> p c n", c=8)
        stage = [A.alloc(8 * 512) for _ in range(2)]
        xt = [A.alloc(D) for _ in range(2 * TPG)]
        hb = [A.alloc(D, BF16) for _ in range(2)]
        hT = [A.alloc(8 * GP, BF16).rearrange("p (c n) -> p c n", c=8) for _ in range(2)]
        vst = [A.alloc(TPG * 1024, BF16).rearrange("p (t n) -> p t n", t=TPG) for _ in range(2)]
        rope = [A.alloc(2 * GP).rearrange("p (w n) -> p w n", w=2) for _ in range(2)]
        t1 = A.alloc(GP)
        t2 = A.alloc(GP)
        junk = A.alloc(D)
        ss = A.alloc(4)
        gm = gcols[:, 24 * l:24 * l + 8]
        for cb in range(12):
            st = stage[cb % 2]
            st3 = st.rearrange("p (c n) -> p c n", c=8)
            dma(st3, w_in_in[l][:, cb * 512:(cb + 1) * 512].rearrange("(c p) n -> p c n", p=128), [], [("stage", cb % 2)], f"stg{cb % 2}")
            eng = pool if cb % 2 == 0 else dve
            eng(lambda e, st3=st3, cb=cb: e.tensor_tensor(out=Wb[:, :, cb * 512:(cb + 1) * 512], in0=st3,
                                                           in1=gm.unsqueeze(2).broadcast_to([128, 8, 512]), op=ALU.mult),
                [("stage", cb % 2), ("gcols", l)], [("Wb", cb)])
        P.barrier()
        fm = [stage[i].bitcast(BF16).rearrange("p (c n) -> p c n", n=GP)[:, 0:32, :] for i in range(2)]
        assert 32 * GP * 2 <= 8 * 512 * 4

        def load_x(g):
            for t in range(TPG):
                tt = g * TPG + t
                b = tt % (2 * TPG)
                dma(xt[b], xsrc[tt * 128:(tt + 1) * 128, :], [("xs", tt)], [("xt", b)], f"xt{b}")
            dma(rope[g % 2], ropeT[:, :, g * GP:(g + 1) * GP].rearrange("w p n -> p w n"), [("ropeT", 0), ("ropeT", 1)], [("rope", g % 2)], f"rope{g % 2}")

        load_x(0)
        COL = dict(sbq=0, sbk=512, sbv=1024, daq=1536, dak=2048, dav=2560, ga=3072, gb=4096, daqp=5120, dakp=5632)
        pb = [0]

        def nb():
            pb[0] = (pb[0] + 1) % 6
            return pb[0]

        for g in range(NGP):
            if g + 1 < NGP:
                load_x(g + 1)
            hTg = hT[g % 2]
            for t in range(TPG):
                tt = g * TPG + t
                b = tt % (2 * TPG)
                x_t = xt[b]
                h_t = hb[tt % 2]
                rstd = ss[:, 1:2]
                rms_rstd(x_t, ss[:, 0:1], rstd, junk, ("xt", b), "rstd1")
                dve(lambda e, x_t=x_t, h_t=h_t: e.tensor_scalar(out=h_t, in0=x_t, scalar1=rstd, scalar2=None, op0=ALU.mult),
                    [("xt", b), "rstd1"], [("hb", tt % 2)])
                tb = bank(6 + tt % 2).bitcast(BF16)
                for c in range(8):
                    pe(lambda e, c=c, tb=tb, h_t=h_t: e.transpose(tb[:, c * 128:(c + 1) * 128], h_t[:, c * 128:(c + 1) * 128], ident_b),
                       [("hb", tt % 2), "ident_b"], [("pb", 6 + tt % 2)])
                act(lambda e, tb=tb, hTg=hTg, t=t: e.activation(out=hTg[:, :, t * 128:(t + 1) * 128], in_=tb.rearrange("p (c n) -> p c n", c=8), func=AF.Copy),
                    [("pb", 6 + tt % 2)], [("hT", g % 2, t)])
            hkeys = [("hT", g % 2, t) for t in range(TPG)]
            fmg = fm[g % 2]

            def proj(col, bk):
                for c in range(8):
                    mm(bank(bk)[:, 0:GP], Wb[:, c, col:col + 128], hTg[:, c, :], c == 0, c == 7,
                       hkeys + [("Wb", col // 512)], [("pb", bk)])

            for k in range(32):
                if k < 4:
                    col, kind = COL["sbq"] + k * 128, "q"
                elif k < 8:
                    col, kind = COL["sbk"] + (k - 4) * 128, "k"
                elif k < 12:
                    col, kind = COL["daq"] + (k - 8) * 128, "rq"
                elif k < 16:
                    col, kind = COL["dak"] + (k - 12) * 128, "rk"
                elif k < 24:
                    col, kind = COL["ga"] + (k - 16) * 128, "s"
                else:
                    col, kind = COL["gb"] + (k - 24) * 128, "s"
                bk = nb()
                proj(col, bk)
                dst = fmg[:, k, :]
                fmk = ("fm", g % 2, k)
                src = bank(bk)[:, 0:GP]
                if kind == "q":
                    act(lambda e, dst=dst, src=src: e.activation(out=dst, in_=src, func=AF.Copy, scale=0.125), [("pb", bk)], [fmk])
                elif kind == "k":
                    act(lambda e, dst=dst, src=src: e.activation(out=dst, in_=src, func=AF.Copy), [("pb", bk)], [fmk])
                elif kind == "s":
                    act(lambda e, dst=dst, src=src: e.activation(out=dst, in_=src, func=AF.Sigmoid), [("pb", bk)], [fmk])
                else:
                    pcol = (COL["daqp"] if kind == "rq" else COL["dakp"]) + (col % 512)
                    bk2 = nb()
                    proj(pcol, bk2)
                    src2 = bank(bk2)[:, 0:GP]
                    rp = rope[g % 2]
                    sc = 0.125 if kind == "rq" else 1.0
                    dve(lambda e, src=src, rp=rp, sc=sc: e.scalar_tensor_tensor(out=t1, in0=src, scalar=sc, in1=rp[:, 0, :], op0=ALU.mult, op1=ALU.mult),
                        [("pb", bk), ("rope", g % 2)], ["t1"])
                    dve(lambda e, src2=src2, rp=rp, sc=sc: e.scalar_tensor_tensor(out=t2, in0=src2, scalar=sc, in1=rp[:, 1, :], op0=ALU.mult, op1=ALU.mult),
                        [("pb", bk2), ("rope", g % 2)], ["t2"])
                    pool(lambda e, dst=dst: e.tensor_tensor(out=dst, in0=t1, in1=t2, op=ALU.add), ["t1", "t2"], [fmk])
            vg = vst[g % 2]
            for t in range(TPG):
                for hv, col in enumerate((COL["sbv"], COL["dav"])):
                    bk = nb()
                    for c in range(8):
                        mm(bank(bk), hTg[:, c, t * 128:(t + 1) * 128], Wb[:, c, col:col + 512], c == 0, c == 7,
                           hkeys + [("Wb", col // 512)], [("pb", bk)])
                    dve(lambda e, vg=vg, t=t, hv=hv, bk=bk: e.tensor_copy(out=vg[:, t, hv * 512:(hv + 1) * 512], in_=bank(bk)), [("pb", bk)], [("vst", g % 2)])
            gs = slice(g * GP, (g + 1) * GP)
            dma(qkT[:, :, gs].rearrange("k p n -> p k n"), fmg[:, 0:16, :], [("fm", g % 2, k) for k in range(16)], [("qkT", g)], f"stg{g % 2}", q="pool")
            dma(sgT[:, :, gs].rearrange("k p n -> p k n"), fmg[:, 16:32, :], [("fm", g % 2, k) for k in range(16, 32)], [("sgT", g)], f"stgb{g % 2}", q="pool")
            dma(vsc[g * GP:(g + 1) * GP, :].rearrange("(t p) n -> p t n", p=128), vg, [("vst", g % 2)], [("vsc", g)], f"vst{g % 2}", q="pool")
        P.barrier()

    def phase_sb(l):
        A.reset()
        Vall = A.alloc(NT * 512, BF16).rearrange("p (k n) -> p k n", n=512)
        KT = [A.alloc(S, BF16, parts=64) for _ in range(2)]
        QT = [A.alloc(S, BF16, parts=64) for _ in range(2)]
        Eb = [A.alloc(512) for _ in range(2)]
        Ln_ = [A.alloc(512, BF16) for _ in range(3)]
        Wt = [A.alloc(512, BF16) for _ in range(3)]
        ost = [A.alloc(512, BF16, parts=64) for _ in range(2)]
        nvd = 4
        for i in range(nvd):
            k0, k1 = NT * i // nvd, NT * (i + 1) // nvd
            dma(Vall[:, k0:k1, :], vsc[k0 * 128:k1 * 128, 0:512].rearrange("(k p) n -> p k n", p=128),
                [("vsc", g) for g in range(S // 256)], [("Vall", i)], f"va{i}")
        vkeys = [("Vall", i) for i in range(nvd)]

        def load_head(h):
            b = h % 2
            r0 = (h % 2) * 64
            allq = [("qkT", g) for g in range(S // 256)]
            dma(KT[b], qkT[4 + h // 2, r0:r0 + 64, :], allq, [("KT", b)], f"kt{b}")
            dma(QT[b], qkT[h // 2, r0:r0 + 64, :], allq, [("QT", b)], f"qt{b}")

        tiles = []
        for h in range(8):
            for qt in range(NG):
                kbs = list(range(4 * qt + 3, -1, -1))
                for i, kb in enumerate(kbs):
                    tiles.append((h, qt, kb, i == 0, i == len(kbs) - 1, kb - 4 * qt if kb >= 4 * qt else -1))
        n = len(tiles)
        chain_of = {}
        ci = -1
        for i, tl in enumerate(tiles):
            if tl[3]:
                ci += 1
            chain_of[i] = ci

        load_head(0)

        def zb(i):
            return i % 4

        def stepA(i):
            h, qt, kb, first, last, dj = tiles[i]
            if first and qt == 0 and h + 1 < 8:
                load_head(h + 1)
            b = h % 2
            mm(bank(zb(i)), KT[b][:, kb * 128:(kb + 1) * 128], QT[b][:, qt * 512:(qt + 1) * 512], True, False,
               [("KT", b), ("QT", b)], [("pb", zb(i))])
            e_ = Eb[i % 2]
            act(lambda e, e_=e_, i=i: e.activation(out=e_, in_=bank(zb(i)), func=AF.Exp), [("pb", zb(i))], [("Eb", i % 2)])

        def stepC(i):
            h, qt, kb, first, last, dj = tiles[i]
            e_ = Eb[i % 2]
            ln = Ln_[i % 3]
            act(lambda e, e_=e_, ln=ln: e.activation(out=ln, in_=e_, func=AF.Ln, bias=1.0), [("Eb", i % 2)], [("Ln", i % 3)])
            if dj >= 0:
                dve(lambda e, ln=ln, dj=dj: e.tensor_tensor(out=ln, in0=ln, in1=msb_b[:, dj * 512:(dj + 1) * 512], op=ALU.mult),
                    [("Ln", i % 3), "msb_b"], [("Ln", i % 3)])

        def stepD(i):
            h, qt, kb, first, last, dj = tiles[i]
            c = chain_of[i]
            ln = Ln_[i % 3]
            mm(bank(zb(i)), tri_b, ln, False, first, [("Ln", i % 3), "tri_b"], [("pb", zb(i))])
            if not first:
                cr = carry[(i - 1) % 2]
                mm(bank(zb(i)), ones2, cr, False, True, [("carry", (i - 1) % 2), "ones2"], [("pb", zb(i))])
            if not last:
                csb = bank(6 + c % 2)[0:2, :]
                mm(csb, tri2, ln, first, False, [("Ln", i % 3), "tri2"], [("cs", c % 2)])
                cr = carry[i % 2]
                dve(lambda e, csb=csb: e.tensor_copy(out=hi2, in_=csb), [("cs", c % 2)], ["hi2"])
                dve(lambda e, cr=cr, csb=csb: e.scalar_tensor_tensor(out=cr, in0=hi2, scalar=negsel, in1=csb, op0=ALU.mult, op1=ALU.add),
                    [("cs", c % 2), "hi2", "ropef"], [("carry", i % 2)])
            w = Wt[i % 3]
            act(lambda e, w=w, i=i: e.activation(out=w, in_=bank(zb(i)), func=AF.Exp), [("pb", zb(i))], [("Wt", i % 3)])
            if dj >= 0:
                dve(lambda e, w=w, dj=dj: e.tensor_tensor(out=w, in0=w, in1=msb_b[:, dj * 512:(dj + 1) * 512], op=ALU.mult),
                    [("Wt", i % 3), "msb_b"], [("Wt", i % 3)])

        def stepG(i):
            h, qt, kb, first, last, dj = tiles[i]
            c = chain_of[i]
            ob = bank(4 + c % 2)[0:64, :]
            w = Wt[i % 3]
            mm(ob, Vall[:, kb, h * 64:(h + 1) * 64], w, first, last, [("Wt", i % 3)] + vkeys, [("ob", c % 2)])
            if last:
                o_ = ost[c % 2]
                dve(lambda e, o_=o_, ob=ob: e.tensor_copy(out=o_, in_=ob), [("ob", c % 2)], [("ost", c % 2)])
                dma(atT[h // 2, (h % 2) * 64:(h % 2) * 64 + 64, qt * 512:(qt + 1) * 512], o_, [("ost", c % 2)], [("atT", 0, h, qt)], f"ost{c % 2}", q="pool")

        for s in range(n + 2):
            if s < n:
                stepA(s)
            if 0 <= s - 1 < n:
                stepD(s - 1)
            if s < n:
                stepC(s)
            if 0 <= s - 2 < n:
                stepG(s - 2)
        P.barrier()

    def phase_da(l):
        A.reset()
        Vall = A.alloc(NT * 512, BF16).rearrange("p (k n) -> p k n", n=512)
        KT = A.alloc(S, BF16)
        QT = A.alloc(S, BF16)
        Pb = [A.alloc(1024, BF16) for _ in range(3)]
        r1 = A.alloc(512)
        o1 = A.alloc(512)
        o2 = A.alloc(512)
        ysq = A.alloc(512, BF16)
        yst = [A.alloc(512, BF16) for _ in range(2)]
        gcomb = A.alloc(1)
        dve(lambda e: e.tensor_scalar(out=gcomb, in0=gsub[:, l:l + 1], scalar1=float(1.0 - lam_inits[l]), scalar2=None, op0=ALU.mult),
            [("gsub", l)], ["gcomb"])
        nvd = 4
        for i in range(nvd):
            k0, k1 = NT * i // nvd, NT * (i + 1) // nvd
            dma(Vall[:, k0:k1, :], vsc[k0 * 128:k1 * 128, 512:1024].rearrange("(k p) n -> p k n", p=128),
                [("vsc", g) for g in range(S // 256)], [("Vall", i)], f"va{i}")
        vkeys = [("Vall", i) for i in range(nvd)]
        allq = [("qkT", g) for g in range(S // 256)]
        cnt = 0
        cq = 0
        for h in range(4):
            dma(KT, qkT[12 + h], allq, ["KTd"], "kt0")
            dma(QT, qkT[8 + h], allq, ["QTd"], "qt0")
            for qt in range(NG):
                nkb = 4 * qt + 4
                qs = slice(qt * 512, (qt + 1) * 512)

                def front(kb, cnt):
                    sb2 = (cnt % 2) * 2
                    ks = slice(kb * 128, (kb + 1) * 128)
                    mm(bank(sb2), KT[0:64, ks], QT[0:64, qs], True, True, ["KTd", "QTd"], [("pb", sb2)])
                    mm(bank(sb2 + 1), KT[64:128, ks], QT[64:128, qs], True, True, ["KTd", "QTd"], [("pb", sb2)])
                    pbuf = Pb[cnt % 3]
                    act(lambda e, pbuf=pbuf, sb2=sb2: e.activation(out=pbuf, in_=bank(sb2, 2), func=AF.Exp), [("pb", sb2)], [("Pb", cnt % 3)])
                    dj = kb - 4 * qt
                    if dj >= 0:
                        for m_ in range(2):
                            dve(lambda e, pbuf=pbuf, dj=dj, m_=m_: e.tensor_tensor(out=pbuf[:, m_ * 512:(m_ + 1) * 512], in0=pbuf[:, m_ * 512:(m_ + 1) * 512],
                                                                                    in1=mda_b[:, dj * 512:(dj + 1) * 512], op=ALU.mult),
                                [("Pb", cnt % 3), "mda_b"], [("Pb", cnt % 3)])

                def back(kb, cnt):
                    pbuf = Pb[cnt % 3]
                    first, last = kb == 0, kb == nkb - 1
                    vv = Vall[:, kb, h * 128:(h + 1) * 128]
                    rk = [("Pb", cnt % 3)] + vkeys
                    mm(bank(4), vv, pbuf[:, 0:512], first, last, rk, [("pb", 4)])
                    mm(bank(5), vv, pbuf[:, 512:1024], first, last, rk, [("pb", 5)])
                    mm(bank(6), ones_b, pbuf[:, 0:512], first, last, rk + ["ones_b"], [("pb", 6)])
                    mm(bank(7), ones_b, pbuf[:, 512:1024], first, last, rk + ["ones_b"], [("pb", 7)])

                import os
                kda = int(os.environ.get("KDA", "3"))
                front(0, cnt)
                for kb in range(nkb):
                    if kb + 1 < nkb:
                        front(kb + 1, cnt + kb + 1)
                    if kda >= 2:
                        back(kb, cnt + kb)
                cnt += nkb
                if kda < 3:
                    continue
                if kda == 10:
                    dve(lambda e: e.reciprocal(out=r1, in_=bank(6)), [("pb", 6)], ["r1"])
                    continue
                if kda == 11:
                    dve(lambda e: e.tensor_copy(out=r1, in_=bank(6)), [("pb", 6)], ["r1"])
                    dve(lambda e: e.reciprocal(out=r1, in_=r1), ["r1"], ["r1"])
                    dve(lambda e: e.tensor_tensor(out=o1, in0=bank(4), in1=r1, op=ALU.mult), [("pb", 4), "r1"], ["o1"])
                    continue
                dve(lambda e: e.reciprocal(out=r1, in_=bank(6)), [("pb", 6)], ["r1"])
                dve(lambda e: e.tensor_tensor(out=o1, in0=bank(4), in1=r1, op=ALU.mult), [("pb", 4), "r1"], ["o1"])
                dve(lambda e: e.reciprocal(out=r1, in_=bank(7)), [("pb", 7)], ["r1"])
                dve(lambda e: e.tensor_tensor(out=o2, in0=bank(5), in1=r1, op=ALU.mult), [("pb", 5), "r1"], ["o2"])
                dve(lambda e: e.scalar_tensor_tensor(out=o1, in0=o2, scalar=neglam[:, l:l + 1], in1=o1, op0=ALU.mult, op1=ALU.add),
                    ["o1", "o2", ("neglam", l)], ["o1"])
                if kda == 12:
                    continue
                dve(lambda e: e.tensor_tensor(out=ysq, in0=o1, in1=o1, op=ALU.mult), ["o1"], ["ysq"])
                if kda == 13:
                    continue
                mm(bank(6), ones_b, ysq, True, True, ["ysq", "ones_b"], [("pb", 6)])
                dve(lambda e: e.tensor_scalar(out=r1, in0=bank(6), scalar1=1.0 / 128, scalar2=EPS, op0=ALU.mult, op1=ALU.add), [("pb", 6)], ["r1"])
                if kda == 14:
                    continue
                act(lambda e: e.activation(out=r1, in_=r1, func=AF.Ln), ["r1"], ["r1"])
                act(lambda e: e.activation(out=r1, in_=r1, func=AF.Exp, scale=-0.5), ["r1"], ["r1"])
                dve(lambda e: e.tensor_tensor(out=o1, in0=o1, in1=r1, op=ALU.mult), ["o1", "r1"], ["o1"])
                y_ = yst[cq % 2]
                dve(lambda e, y_=y_: e.tensor_scalar(out=y_, in0=o1, scalar1=gcomb, scalar2=None, op0=ALU.mult), ["o1", "gcomb"], [("yst", cq % 2)])
                dma(atT[4 + h, :, qs], y_, [("yst", cq % 2)], [("atT", 1, h, qt)], f"yst{cq % 2}", q="pool")
                cq += 1
        P.barrier()

    def phase_merge(l):
        A.reset()
        xsrc = x_in if l == 0 else xs
        Wa = A.alloc(4 * D, BF16).rearrange("p (c n) -> p c n", c=4)
        Wbb = A.alloc(4 * D, BF16).rearrange("p (c n) -> p c n", c=4)
        Wo = A.alloc(8 * D, BF16).rearrange("p (c n) -> p c n", c=8)
        Wr = A.alloc(8 * 36).rearrange("p (c n) -> p c n", c=8)
        stage = A.alloc(4 * D)
        load_cast(Wa, w_a_in[l], 4, D, stage, ["stg4"], "Wa", "stg0", eng="pool")
        load_cast(Wbb, w_b_in[l], 4, D, stage, ["stg4"], "Wbb", "stg0", eng="pool")
        load_cast(Wo[:, 0:4, :], w_o_in[l][0:512, :], 4, D, stage, ["stg4"], "Wo", "stg0", eng="pool")
        load_cast(Wo[:, 4:8, :], w_o_in[l][512:1024, :], 4, D, stage, ["stg4"], "Wo2", "stg0", eng="pool")
        dma(Wr, w_r_in[l].rearrange("(c p) n -> p c n", p=128), [], ["Wr0"], "c0")
        gf = gcols[:, 24 * l + 8:24 * l + 16]
        dve(lambda e: e.tensor_tensor(out=Wr, in0=Wr, in1=gf.unsqueeze(2).broadcast_to([128, 8, 36]), op=ALU.mult), ["Wr0", ("gcols", l)], ["Wr"])
        aT = [A.alloc(4 * 512, BF16).rearrange("p (c n) -> p c n", c=4) for _ in range(2)]
        bT = [A.alloc(4 * 512, BF16).rearrange("p (c n) -> p c n", c=4) for _ in range(2)]
        sA = [A.alloc(16 * 512, BF16).rearrange("p (c n) -> p c n", c=16) for _ in range(2)]
        mT = A.alloc(8 * 512, BF16).rearrange("p (c n) -> p c n", c=8)
        t1 = A.alloc(512)
        t2 = A.alloc(512)
        xt = [A.alloc(D) for _ in range(2)]
        hf = A.alloc(D)
        hbb = A.alloc(D, BF16)
        hfT = A.alloc(D)
        h2st = [A.alloc(8 * 512, BF16).rearrange("p (c n) -> p c n", c=8) for _ in range(2)]
        junk = A.alloc(D)
        ss = A.alloc(4)
        rw = A.alloc(36 * 8)
        brl = brt[:, 36 * l:36 * l + 36]

        def load_g(g):
            gs = slice(g * 512, (g + 1) * 512)
            b = g % 2
            dma(aT[b], atT[0:4, :, gs].rearrange("k p n -> p k n"), [("atT", 0, h, g) for h in range(8)], [("aT", b)], f"aT{b}")
            dma(bT[b], atT[4:8, :, gs].rearrange("k p n -> p k n"), [("atT", 1, h, g) for h in range(4)], [("bT", b)], f"bT{b}")
            dma(sA[b], sgT[:, :, gs].rearrange("k p n -> p k n"), [("sgT", 2 * g), ("sgT", 2 * g + 1)], [("sA", b)], f"sA{b}")

        load_g(0)
        for g in range(NG):
            if g + 1 < NG:
                load_g(g + 1)
            b = g % 2
            for oc in range(8):
                pa = (oc % 2) * 2
                for k in range(4):
                    mm(bank(pa), Wa[:, k, oc * 128:(oc + 1) * 128], aT[b][:, k, :], k == 0, k == 3, ["Wa", ("aT", b)], [("pb", pa)])
                for k in range(4):
                    mm(bank(pa + 1), Wbb[:, k, oc * 128:(oc + 1) * 128], bT[b][:, k, :], k == 0, k == 3, ["Wbb", ("bT", b)], [("pb", pa + 1)])
                dve(lambda e, pa=pa, oc=oc, b=b: e.tensor_tensor(out=t1, in0=bank(pa), in1=sA[b][:, oc, :], op=ALU.mult), [("pb", pa), ("sA", b)], ["t1"])
                dve(lambda e, pa=pa, oc=oc, b=b: e.tensor_tensor(out=t2, in0=bank(pa + 1), in1=sA[b][:, 8 + oc, :], op=ALU.mult), [("pb", pa + 1), ("sA", b)], ["t2"])
                pool(lambda e, oc=oc: e.tensor_tensor(out=mT[:, oc, :], in0=t1, in1=t2, op=ALU.add), ["t1", "t2"], [("mT", oc)])
            mkeys = [("mT", oc) for oc in range(8)]
            for t in range(4):
                tt = g * 4 + t
                xb_ = xt[tt % 2]
                dma(xb_, xsrc[tt * 128:(tt + 1) * 128, :], [("xs", tt)], [("xt", tt % 2)], f"xt{tt % 2}")
                for half in range(2):
                    for oc in range(8):
                        mm(bank(4 + half), mT[:, oc, t * 128:(t + 1) * 128], Wo[:, oc, half * 512:(half + 1) * 512], oc == 0, oc == 7,
                           mkeys + ["Wo", "Wo2"], [("pb", 4 + half)])
                dve(lambda e, xb_=xb_: e.tensor_tensor(out=xb_, in0=xb_, in1=bank(4, 2), op=ALU.add), [("xt", tt % 2), ("pb", 4), ("pb", 5)], [("xt", tt % 2)])
                dma(xs[tt * 128:(tt + 1) * 128, :], xb_, [("xt", tt % 2)], [("xs", tt)], f"xo{tt % 2}", q="pool")
                rstd = ss[:, 1:2]
                rms_rstd(xb_, ss[:, 0:1], rstd, junk, ("xt", tt % 2), "rstd4")
                dve(lambda e, xb_=xb_: e.tensor_scalar(out=hf, in0=xb_, scalar1=rstd, scalar2=None, op0=ALU.mult), [("xt", tt % 2), "rstd4"], ["hf"])
                pool(lambda e: e.tensor_copy(out=hbb, in_=hf), ["hf"], ["hbb"])
                tb = bank(6).bitcast(BF16)
                for c in range(8):
                    pe(lambda e, c=c, tb=tb: e.transpose(tb[:, c * 128:(c + 1) * 128], hbb[:, c * 128:(c + 1) * 128], ident_b), ["hbb", "ident_b"], [("pb", 6)])
                h2g = h2st[g % 2]
                act(lambda e, tb=tb, h2g=h2g, t=t: e.activation(out=h2g[:, :, t * 128:(t + 1) * 128], in_=tb.rearrange("p (c n) -> p c n", c=8), func=AF.Copy),
                    [("pb", 6)], [("h2st", g % 2)])
                for hh in range(2):
                    for c in range(4):
                        cc = hh * 4 + c
                        pe(lambda e, c=c, cc=cc: e.transpose(bank(7)[:, c * 128:(c + 1) * 128], hf[:, cc * 128:(cc + 1) * 128], ident_f), ["hf", "ident_f"], [("pb", 7)])
                    act(lambda e, hh=hh: e.activation(out=hfT[:, hh * 512:(hh + 1) * 512], in_=bank(7), func=AF.Copy), [("pb", 7)], [("hfT", hh)])
                lg = bank(4)[:, 0:36]
                for c in range(8):
                    mm(lg, hfT[:, c * 128:(c + 1) * 128], Wr[:, c, :], c == 0, c == 7, [("hfT", 0), ("hfT", 1), "Wr"], [("pb", 4)])
                lgs = rw[:, 0:36]
                gmax = rw[:, 36:37]
                oh = rw[:, 40:44]
                pen = rw[:, 44:48]
                eg = rw[:, 48:52]
                gsum = rw[:, 52:53]
                els = rw[:, 56:88]
                emax = rw[:, 88:89]
                negm = rw[:, 89:90]
                ee = rw[:, 96:128]
                m1 = rw[:, 128:129]
                mk1 = rw[:, 136:168]
                ee2 = rw[:, 168:200]
                m2 = rw[:, 200:201]
                mk2 = rw[:, 208:240]
                den = rw[:, 240:241]
                Gd = Gt[:, tt * 32:(tt + 1) * 32]
                R = "rw"

                def dv(fn, extra_r=(), w=(R,)):
                    dve(fn, [R] + list(extra_r), list(w))

                dve(lambda e: e.tensor_tensor(out=lgs, in0=lg, in1=brl, op=ALU.add), [("pb", 4), ("brt", l)], [R])
                dv(lambda e: e.reduce_max(out=gmax, in_=lgs[:, 0:4], axis=mybir.AxisListType.X))
                dv(lambda e: e.tensor_scalar(out=oh, in0=lgs[:, 0:4], scalar1=gmax, scalar2=None, op0=ALU.is_ge))
                dv(lambda e: e.tensor_scalar(out=pen, in0=oh, scalar1=-1.0, scalar2=1e30, op0=ALU.add, op1=ALU.mult))
                dv(lambda e: e.tensor_scalar(out=negm, in0=gmax, scalar1=-1.0, scalar2=None, op0=ALU.mult))
                act(lambda e: e.activation(out=eg, in_=lgs[:, 0:4], func=AF.Exp, bias=negm, accum_out=gsum), [R], [R])
                for gi in range(4):
                    dv(lambda e, gi=gi: e.tensor_scalar(out=els[:, gi * 8:(gi + 1) * 8], in0=lgs[:, 4 + gi * 8:4 + (gi + 1) * 8], scalar1=pen[:, gi:gi + 1], scalar2=None, op0=ALU.add))
                dv(lambda e: e.reduce_max(out=emax, in_=els, axis=mybir.AxisListType.X))
                dv(lambda e: e.tensor_scalar(out=negm, in0=emax, scalar1=-1.0, scalar2=None, op0=ALU.mult))
                act(lambda e: e.activation(out=ee, in_=els, func=AF.Exp, bias=negm), [R], [R])
                dv(lambda e: e.reduce_max(out=m1, in_=ee, axis=mybir.AxisListType.X))
                dv(lambda e: e.tensor_scalar(out=mk1, in0=ee, scalar1=m1, scalar2=None, op0=ALU.is_ge))
                dv(lambda e: e.scalar_tensor_tensor(out=ee2, in0=mk1, scalar=-2.0, in1=ee, op0=ALU.mult, op1=ALU.add))
                dv(lambda e: e.reduce_max(out=m2, in_=ee2, axis=mybir.AxisListType.X))
                dv(lambda e: e.tensor_scalar(out=mk2, in0=ee2, scalar1=m2, scalar2=None, op0=ALU.is_ge))
                dv(lambda e: e.tensor_tensor(out=den, in0=m1, in1=m2, op=ALU.add))
                dv(lambda e: e.tensor_tensor(out=den, in0=den, in1=gsum, op=ALU.mult))
                dv(lambda e: e.reciprocal(out=den, in_=den))
                dv(lambda e: e.tensor_scalar(out=mk1, in0=mk1, scalar1=m1, scalar2=den, op0=ALU.mult, op1=ALU.mult))
                dv(lambda e: e.tensor_scalar(out=mk2, in0=mk2, scalar1=m2, scalar2=den, op0=ALU.mult, op1=ALU.mult))
                dve(lambda e, Gd=Gd: e.tensor_tensor(out=Gd, in0=mk1, in1=mk2, op=ALU.add), [R], [("Gt", tt)])
            dma(h2T[:, :, g * 512:(g + 1) * 512].rearrange("k p n -> p k n"), h2st[g % 2], [("h2st", g % 2)], [("h2T", g)], f"h2st{g % 2}", q="pool")
        P.barrier()

    def phase_moe(l):
        A.reset()
        TG = min(S, 2048)
        NTG = TG // 128
        hTg = A.alloc(8 * TG, BF16).rearrange("p (c n) -> p c n", c=8)
        acc = A.alloc(NTG * D).rearrange("p (t n) -> p t n", t=NTG)
        Wgu = [A.alloc(8 * 512, BF16).rearrange("p (c n) -> p c n", c=8) for _ in range(2)]
        Wd = [A.alloc(2 * D, BF16).rearrange("p (c n) -> p c n", c=2) for _ in range(2)]
        stg = A.alloc(8 * 256)
        stu = A.alloc(8 * 256)
        std = A.alloc(2 * D)
        sl = [A.alloc(256) for _ in range(2)]
        ab = [A.alloc(256, BF16) for _ in range(2)]
        aT = [A.alloc(256, BF16) for _ in range(2)]
        gf = gcols[:, 24 * l + 8:24 * l + 16]

        def load_w(e, b):
            dma(stg.rearrange("p (c n) -> p c n", c=8), w_eg_in[l, e].rearrange("(c p) n -> p c n", p=128), [], ["stg"], "stg0")
            dma(stu.rearrange("p (c n) -> p c n", c=8), w_eu_in[l, e].rearrange("(c p) n -> p c n", p=128), [], ["stu"], "stg1")
            dma(std.rearrange("p (c n) -> p c n", c=2), w_ed_in[l, e].rearrange("(c p) n -> p c n", p=128), [], ["std"], "stg2")
            pool(lambda en: en.tensor_tensor(out=Wgu[b][:, :, 0:256], in0=stg.rearrange("p (c n) -> p c n", c=8),
                                             in1=gf.unsqueeze(2).broadcast_to([128, 8, 256]), op=ALU.mult), ["stg", ("gcols", l)], [("Wgu", b, 0)])
            pool(lambda en: en.tensor_tensor(out=Wgu[b][:, :, 256:512], in0=stu.rearrange("p (c n) -> p c n", c=8),
                                             in1=gf.unsqueeze(2).broadcast_to([128, 8, 256]), op=ALU.mult), ["stu", ("gcols", l)], [("Wgu", b, 1)])
            pool(lambda en: en.tensor_copy(out=Wd[b], in_=std.rearrange("p (c n) -> p c n", c=2)), ["std"], [("Wd", b)])

        for tg in range(S // TG):
            t0 = tg * NTG
            dma(hTg, h2T[:, :, tg * TG:(tg + 1) * TG].rearrange("k p n -> p k n"), [("h2T", g) for g in range(tg * TG // 512, (tg + 1) * TG // 512)], ["hTg"], "hTg")
            dma(acc, xs[t0 * 128:(t0 + NTG) * 128, :].rearrange("(t p) n -> p t n", p=128), [("xs", t0 + t) for t in range(NTG)],
                [("acc", t) for t in range(NTG)], "accl")
            load_w(0, 0)
            load_w(1, 1)
            items = [(e, t) for e in range(NE) for t in range(NTG)]

            def front(i):
                e, t = items[i]
                b = e % 2
                hb_ = i % 2
                for c in range(8):
                    mm(bank(hb_), hTg[:, c, t * 128:(t + 1) * 128], Wgu[b][:, c, :], c == 0, c == 7, ["hTg", ("Wgu", b, 0), ("Wgu", b, 1)], [("pb", hb_)])
                s_ = sl[i % 2]
                a_ = ab[i % 2]
                act(lambda en, s_=s_, hb_=hb_: en.activation(out=s_, in_=bank(hb_)[:, 0:256], func=AF.Silu), [("pb", hb_)], [("sl", i % 2)])
                gcolumn = Gt[:, (t0 + t) * 32 + e:(t0 + t) * 32 + e + 1]
                dve(lambda en, s_=s_, a_=a_, hb_=hb_, gcolumn=gcolumn: en.scalar_tensor_tensor(out=a_, in0=s_, scalar=gcolumn, in1=bank(hb_)[:, 256:512], op0=ALU.mult, op1=ALU.mult),
                    [("sl", i % 2), ("pb", hb_), ("Gt", t0 + t)], [("ab", i % 2)])

            def back(i):
                e, t = items[i]
                b = e % 2
                a_ = ab[i % 2]
                tb = bank(2 + i % 2).bitcast(BF16)
                for c in range(2):
                    pe(lambda en, c=c, tb=tb, a_=a_: en.transpose(tb[:, c * 128:(c + 1) * 128], a_[:, c * 128:(c + 1) * 128], ident_b), [("ab", i % 2), "ident_b"], [("pb", 2 + i % 2)])
                at_ = aT[i % 2]
                act(lambda en, at_=at_, tb=tb: en.activation(out=at_, in_=tb[:, 0:256], func=AF.Copy), [("pb", 2 + i % 2)], [("aT", i % 2)])
                ob = 4 + (i % 2) * 2
                for half in range(2):
                    for c in range(2):
                        mm(bank(ob + half), at_[:, c * 128:(c + 1) * 128], Wd[b][:, c, half * 512:(half + 1) * 512], c == 0, c == 1, [("aT", i % 2), ("Wd", b)], [("pb", ob + half)])
                dve(lambda en, t=t, ob=ob: en.tensor_tensor(out=acc[:, t, :], in0=acc[:, t, :], in1=bank(ob, 2), op=ALU.add), [("acc", t), ("pb", ob), ("pb", ob + 1)], [("acc", t)])

            front(0)
            for i in range(len(items)):
                if i + 1 < len(items):
                    front(i + 1)
                back(i)
                if items[i][1] == NTG - 1 and items[i][0] + 2 < NE:
                    load_w(items[i][0] + 2, items[i][0] % 2)
            dma(xs[t0 * 128:(t0 + NTG) * 128, :].rearrange("(t p) n -> p t n", p=128), acc, [("acc", t) for t in range(NTG)],
                [("xs", t0 + t) for t in range(NTG)], "accs", q="pool")
        P.barrier()

    def phase_ple(l):
        A.reset()
        last = l == L - 1
        Wpg = A.alloc(8 * D, BF16).rearrange("p (c n) -> p c n", c=8)
        Wpl = A.alloc(2 * D, BF16).rearrange("p (c n) -> p c n", c=2)
        stage = A.alloc(8 * D)
        gp = gcols[:, 24 * l + 16:24 * l + 24]
        load_cast(Wpg, w_pg_in[l], 8, D, stage, ["stg6"], "Wpg", "stg0", gcol=gp, gkey=("gcols", l), eng="pool")
        load_cast(Wpl, w_ple_in[l], 2, D, stage[:, 0:2 * D], ["stg6"], "Wpl", "stg0", eng="dve")
        xt = [A.alloc(D) for _ in range(2)]
        pt = [A.alloc(256) for _ in range(2)]
        hb = A.alloc(D, BF16)
        pbf = A.alloc(256, BF16)
        hT = A.alloc(D, BF16)
        pT = A.alloc(256, BF16)
        sg = A.alloc(D)
        junk = A.alloc(D)
        ss = A.alloc(4)
        ot = [A.alloc(D) for _ in range(2)]
        for tt in range(NT):
            b = tt % 2
            dma(xt[b], xs[tt * 128:(tt + 1) * 128, :], [("xs", tt)], [("xt", b)], f"xt{b}")
            dma(pt[b], p_in[l, tt * 128:(tt + 1) * 128, :], [], [("pt", b)], f"pt{b}")
            rstd = ss[:, 1:2]
            rms_rstd(xt[b], ss[:, 0:1], rstd, junk, ("xt", b), "rstd6")
            dve(lambda e, b=b: e.tensor_scalar(out=hb, in0=xt[b], scalar1=rstd, scalar2=None, op0=ALU.mult), [("xt", b), "rstd6"], ["hb"])
            pool(lambda e, b=b: e.tensor_copy(out=pbf, in_=pt[b]), [("pt", b)], ["pbf"])
            tb = bank(6).bitcast(BF16)
            for c in range(8):
                pe(lambda e, c=c, tb=tb: e.transpose(tb[:, c * 128:(c + 1) * 128], hb[:, c * 128:(c + 1) * 128], ident_b), ["hb", "ident_b"], [("pb", 6)])
            act(lambda e, tb=tb: e.activation(out=hT, in_=tb, func=AF.Copy), [("pb", 6)], ["hT"])
            tb2 = bank(7).bitcast(BF16)
            for c in range(2):
                pe(lambda e, c=c, tb2=tb2: e.transpose(tb2[:, c * 128:(c + 1) * 128], pbf[:, c * 128:(c + 1) * 128], ident_b), ["pbf", "ident_b"], [("pb", 7)])
            act(lambda e, tb2=tb2: e.activation(out=pT, in_=tb2[:, 0:256], func=AF.Copy), [("pb", 7)], ["pT"])
            for half in range(2):
                for c in range(8):
                    mm(bank(half), hT[:, c * 128:(c + 1) * 128], Wpg[:, c, half * 512:(half + 1) * 512], c == 0, c == 7, ["hT", "Wpg"], [("pb", half)])
            act(lambda e: e.activation(out=sg, in_=bank(0, 2), func=AF.Sigmoid), [("pb", 0), ("pb", 1)], ["sg"])
            for half in range(2):
                for c in range(2):
                    mm(bank(2 + half), pT[:, c * 128:(c + 1) * 128], Wpl[:, c, half * 512:(half + 1) * 512], c == 0, c == 1, ["pT", "Wpl"], [("pb", 2 + half)])
            dve(lambda e: e.tensor_tensor(out=sg, in0=sg, in1=bank(2, 2), op=ALU.mult), ["sg", ("pb", 2), ("pb", 3)], ["sg"])
            o_ = ot[b]
            dve(lambda e, b=b, o_=o_: e.tensor_tensor(out=o_, in0=xt[b], in1=sg, op=ALU.add), [("xt", b), "sg"], [("ot", b)])
            if not last:
                dma(xs[tt * 128:(tt + 1) * 128, :], o_, [("ot", b)], [("xs", tt)], f"ot{b}", q="pool")
            else:
                rstd2 = ss[:, 3:4]
                rms_rstd(o_, ss[:, 2:3], rstd2, junk, ("ot", b), "rstd7")
                dve(lambda e, o_=o_: e.scalar_tensor_tensor(out=o_, in0=o_, scalar=rstd2, in1=gfin_bc, op0=ALU.mult, op1=ALU.mult), [("ot", b), "rstd7", "gfin"], [("ot", b)])
                dma(out_d[tt * 128:(tt + 1) * 128, :], o_, [("ot", b)], [("out", tt)], f"ot{b}", q="pool")
        P.barrier()

    P.barrier()
    import os
    nstop = int(os.environ.get("KSTOP", "99"))
    plist = [phase_rope]
    for l in range(L):
        for ph in (phase_inproj, phase_sb, phase_da, phase_merge, phase_moe, phase_ple):
            plist.append(lambda ph=ph, l=l: ph(l))
    for ph in plist[:nstop]:
        ph()
    P.finalize()

    slots = sorted(P.slotcnt.keys())
    sems = {}
    for e in Prog.CE:
        for ph in range(P.epoch[e] + 1):
            if any(op.needs_inc and op.phase == ph for op in P.ops[e]):
                sems[(e, ph)] = es.enter_context(nc.semaphore(f"s_{e}_{ph}"))
    for s in slots:
        sems[("dma", s)] = es.enter_context(nc.semaphore(f"d_{s}"))
    block = es.enter_context(nc.Block())

    def emit(engobj, ename):
        for op in P.ops[ename]:
            for k, v in op.waits:
                engobj.wait_ge(sems[k], v)
            if op.fn is None:
                continue
            ins = op.fn(engobj)
            if op.slot is not None:
                ins.then_inc(sems[("dma", op.slot)], 16)
            elif op.needs_inc:
                ins.then_inc(sems[(ename, op.phase)], 1)

    @block.sync
    def _(e):
        emit(e, "sp")

    @block.tensor
    def _(e):
        emit(e, "pe")

    @block.scalar
    def _(e):
        emit(e, "act")

    @block.vector
    def _(e):
        emit(e, "dve")

    @block.gpsimd
    def _(e):
        emit(e, "pool")

    es.close()
    return nc, len(sems), {k: len(v) for k, v in P.ops.items()}


def _perm_cols():
    idx = np.arange(512)
    i = idx % 64
    partner = np.where(i < 8, idx + 8, np.where(i < 16, idx - 8, idx))
    return partner


def make_in_maps(inputs, S, L, n_cores):
    f = lambda a: np.ascontiguousarray(np.asarray(a))
    x = f(inputs["x"])
    B = x.shape[0]
    pc = _perm_cols()
    w_in = f(inputs["w_in"])
    w_ext = np.concatenate([w_in, w_in[:, :, 1536 + pc], w_in[:, :, 2048 + pc]], axis=2)
    gcols = np.concatenate([f(inputs[k]).reshape(L, 8, 128).transpose(0, 2, 1) for k in ("g_mix", "g_ffn", "g_ple")], axis=2)
    lam = np.concatenate([f(inputs[k]) for k in ("lam_q1", "lam_k1", "lam_q2", "lam_k2")], axis=1).reshape(L, 1, 256)
    w_r = np.concatenate([f(inputs["w_router_group"]), f(inputs["w_router_expert"])], axis=2)
    b_r = np.concatenate([f(inputs["b_router_group"]), f(inputs["b_router_expert"])], axis=1).reshape(L, 1, 36)
    shared = {
        "consts": _consts_host(S),
        "gcols": np.ascontiguousarray(gcols, dtype=np.float32),
        "w_in": np.ascontiguousarray(w_ext, dtype=np.float32),
        "lam": np.ascontiguousarray(lam, dtype=np.float32),
        "gsub": f(inputs["g_subln"]).reshape(L, 128, 1),
        "w_a": f(inputs["w_br_a"]), "w_b": f(inputs["w_br_b"]), "w_o": f(inputs["w_o"]),
        "w_r": np.ascontiguousarray(w_r, dtype=np.float32), "b_r": np.ascontiguousarray(b_r, dtype=np.float32),
        "w_eg": f(inputs["w_exp_gate"]), "w_eu": f(inputs["w_exp_up"]), "w_ed": f(inputs["w_exp_down"]),
        "w_ple": f(inputs["w_ple"]), "w_pg": f(inputs["w_ple_gate"]),
        "gfin": f(inputs["g_final"]).reshape(1, D),
    }
    p = f(inputs["p"])
    pos = f(inputs["positions"]).astype(np.int32)
    maps = []
    for c in range(n_cores):
        b = (c * B) // n_cores
        m = dict(shared)
        m["x"] = np.ascontiguousarray(x[b])
        m["p"] = np.ascontiguousarray(p[:, b])
        m["pos"] = np.ascontiguousarray(pos[b].reshape(1, S))
        maps.append(m)
    return maps


_CACHE = {}
_LAST = None


def kernel(**inputs):
    x = np.asarray(inputs["x"])
    B, S, _ = x.shape
    L = np.asarray(inputs["g_mix"]).shape[0]
    lam_inits = [0.8 - 0.6 * math.exp(-0.3 * i) for i in range(L)]
    key = (S, L)
    if key not in _CACHE:
        _CACHE[key] = build_program(S, L, lam_inits)[0]
    nc = _CACHE[key]
    maps = make_in_maps(inputs, S, L, N_CORES)
    res = run_bass_kernel_spmd(nc, maps, core_ids=list(range(N_CORES)))
    global _LAST
    _LAST = res.results
    per = N_CORES // B
    out = np.stack([np.asarray(res.results[b * per]["out"]) for b in range(B)], axis=0)
    return out.astype(np.float32)
```

```python
import math
import numpy as np
import concourse.bass as bass
import concourse.mybir as mybir
from concourse.bass_utils import run_bass_kernel_spmd

F32, BF16, I32 = mybir.dt.float32, mybir.dt.bfloat16, mybir.dt.int32
AF = mybir.ActivationFunctionType
ALU = mybir.AluOpType

D = 1024
NE = 32
EH = 256
EPS = 1e-6
TWO_PI = 2.0 * math.pi
N_CORES = 8


class _Op:
    __slots__ = ("eng", "fn", "deps", "needs_inc", "cnt", "phase", "slot", "dmacnt", "waits")


class Prog:
    CE = ("pe", "act", "dve", "pool")
    ALL = ("pe", "act", "dve", "pool", "sp")

    def __init__(self):
        self.ops = {e: [] for e in self.ALL}
        self.lw = {}
        self.rd = {}
        self.phase = 0
        self.slotcnt = {}
        self.pending_dma = []
        self.slotmap = {}
        self.epoch = {e: 0 for e in self.ALL}
        self.opcount = {e: 0 for e in self.ALL}

    def add(self, eng, fn, reads=(), writes=(), slot=None):
        if slot is not None:
            if slot not in self.slotmap:
                self.slotmap[slot] = "g%d" % len(self.slotmap)
            slot = self.slotmap[slot]
        op = _Op()
        op.eng, op.fn, op.phase, op.slot = eng, fn, self.epoch[eng], slot
        self.opcount[eng] += 1
        op.needs_inc = False
        op.cnt = 0
        op.dmacnt = 0
        deps = []
        raw = set()
        for r in reads:
            w = self.lw.get(r)
            if w is not None:
                deps.append(w)
                raw.add(id(w))
        for r in writes:
            w = self.lw.get(r)
            if w is not None:
                deps.append(w)
            rr = self.rd.get(r)
            if rr:
                deps.extend(rr[0].values())
                deps.extend(rr[1])
        is_dma = slot is not None
        dd = []
        seen = set()
        for d in deps:
            if id(d) in seen:
                continue
            seen.add(id(d))
            if d.slot is not None:
                dd.append(d)
            elif d.eng != eng or is_dma or (eng != "pe" and id(d) in raw):
                d.needs_inc = True
                dd.append(d)
        op.deps = dd
        for r in reads:
            rr = self.rd.get(r)
            if rr is None:
                rr = self.rd[r] = ({}, [])
            if is_dma:
                rr[1].append(op)
            else:
                rr[0][eng] = op
        for r in writes:
            self.lw[r] = op
            self.rd[r] = ({}, [])
        if is_dma:
            self.slotcnt[slot] = self.slotcnt.get(slot, 0) + 1
            op.dmacnt = 16 * self.slotcnt[slot]
            self.pending_dma.append(op)
        self.ops[eng].append(op)
        return op

    def barrier(self):
        lasts = []
        for e in self.CE:
            for o in reversed(self.ops[e]):
                if o.slot is None:
                    if o.fn is not None:
                        lasts.append(o)
                    break
        pend = self.pending_dma
        self.pending_dma = []
        for e in self.ALL:
            op = _Op()
            op.eng, op.fn, op.phase, op.slot = e, None, self.epoch[e], None
            op.needs_inc = False
            op.cnt = 0
            op.dmacnt = 0
            op.deps = []
            for d in lasts:
                if d.eng != e and d.fn is not None:
                    d.needs_inc = True
                    op.deps.append(d)
            op.deps.extend(pend)
            self.ops[e].append(op)
        self.phase += 1
        self.slotmap = {}
        for e in self.ALL:
            if self.opcount[e] > 12000:
                self.epoch[e] += 1
                self.opcount[e] = 0

    def finalize(self):
        for e in self.ALL:
            cnt = {}
            for op in self.ops[e]:
                if op.slot is not None or op.fn is None:
                    op.needs_inc = False
                if op.needs_inc:
                    cnt[op.phase] = cnt.get(op.phase, 0) + 1
                    op.cnt = cnt[op.phase]
                    assert op.cnt < 30000, "semaphore count overflow"
        for s, c in self.slotcnt.items():
            assert 16 * c < 32000, f"dma slot {s} overflow {c}"
        for e in self.ALL:
            waited = {}
            for op in self.ops[e]:
                need = {}
                for d in op.deps:
                    if d.slot is not None:
                        k, v = ("dma", d.slot), d.dmacnt
                    else:
                        k, v = (d.eng, d.phase), d.cnt
                    if waited.get(k, 0) >= v:
                        continue
                    if need.get(k, 0) < v:
                        need[k] = v
                for k, v in need.items():
                    waited[k] = v
                op.waits = list(need.items())


def _consts_host(S):
    c = {}
    c["ident"] = np.eye(128, dtype=np.float32)
    j = np.arange(128)[:, None]
    s = np.arange(128)[None, :]
    c["trineg"] = -(j >= s).astype(np.float32)
    c["ones"] = np.ones((128, 128), np.float32)
    t = np.arange(512)[None, :]
    sb = [((128 * jj + np.arange(128)[:, None]) < t).astype(np.float32) for jj in range(4)]
    da = [(((128 * jj + np.arange(128)[:, None]) // 64) <= (t // 64)).astype(np.float32) for jj in range(4)]
    c["msb"] = np.concatenate(sb, axis=1)
    c["mda"] = np.concatenate(da, axis=1)
    inv_freq = (500000.0 ** (-np.arange(0, 16, 2, dtype=np.float32) / 16.0)).astype(np.float32)
    f = np.zeros((128,), np.float32)
    sg = np.zeros((128,), np.float32)
    for p in range(128):
        i = p % 64
        if i < 8:
            f[p] = inv_freq[i]
            sg[p] = -1.0
        elif i < 16:
            f[p] = inv_freq[i - 8]
            sg[p] = 1.0
    ns = np.zeros((128,), np.float32)
    ns[1] = -1.0
    c["ropef"] = np.stack([f, sg, ns], axis=1)
    return np.concatenate([c["ident"], c["trineg"], c["ones"], c["msb"], c["mda"], c["ropef"]], axis=1).astype(np.float32)


C_IDENT, C_TRI, C_ONES, C_MSB, C_MDA, C_ROPE = 0, 128, 256, 384, 384 + 2048, 384 + 4096
C_TOTAL = 384 + 4096 + 3


def build_program(S, L, lam_inits, debug=False):
    import os
    debug = debug or os.environ.get("KDEBUG") == "1"
    NT = S // 128
    NG = S // 512
    nc = bass.Bass("TRN2", target_bir_lowering=False)
    P = Prog()

    def din(name, shape, dt=F32):
        return nc.dram_tensor(name, list(shape), dt, kind="ExternalInput").ap()

    def dscr(name, shape, dt):
        kind = "ExternalOutput" if debug else "Internal"
        return nc.dram_tensor(name, list(shape), dt, kind=kind).ap()

    x_in = din("x", [S, D])
    p_in = din("p", [L, S, 256])
    pos_in = din("pos", [1, S], I32)
    consts_in = din("consts", [128, C_TOTAL])
    gcols_in = din("gcols", [L, 128, 24])
    w_in_in = din("w_in", [L, D, 6144])
    lam_in = din("lam", [L, 1, 256])
    gsub_in = din("gsub", [L, 128, 1])
    w_a_in = din("w_a", [L, 512, D])
    w_b_in = din("w_b", [L, 512, D])
    w_o_in = din("w_o", [L, D, D])
    w_r_in = din("w_r", [L, D, 36])
    b_r_in = din("b_r", [L, 1, 36])
    w_eg_in = din("w_eg", [L, NE, D, EH])
    w_eu_in = din("w_eu", [L, NE, D, EH])
    w_ed_in = din("w_ed", [L, NE, EH, D])
    w_ple_in = din("w_ple", [L, 256, D])
    w_pg_in = din("w_pg", [L, D, D])
    gfin_in = din("gfin", [1, D])
    out_d = nc.dram_tensor("out", [S, D], F32, kind="ExternalOutput").ap()

    xs = dscr("xs", [S, D], F32)
    qkT = dscr("qkT", [16, 128, S], BF16)
    sgT = dscr("sgT", [16, 128, S], BF16)
    vsc = dscr("vsc", [S, 1024], BF16)
    atT = dscr("atT", [8, 128, S], BF16)
    h2T = dscr("h2T", [8, 128, S], BF16)
    ropeT = dscr("ropeT", [2, 128, S], F32)

    ARENA_W = 52400
    import contextlib
    es = contextlib.ExitStack()
    arena = es.enter_context(nc.sbuf_tensor("arena", [128, ARENA_W], F32))
    psum = es.enter_context(nc.psum_tensor("psum", [128, 4096], F32))

    def bank(b, n=1):
        return psum[:, b * 512:(b + n) * 512]

    class Arena:
        def __init__(self):
            self.off = 0
            self.mark = 0

        def alloc(self, nelem, dt=F32, parts=128):
            nb = nelem * (4 if dt in (F32, I32) else 2)
            w = (nb + 3) // 4
            assert self.off + w <= ARENA_W, f"arena overflow {self.off + w}"
            a = arena[0:parts, self.off:self.off + w]
            self.off += w
            if dt != F32:
                a = a.bitcast(dt)
            return a

        def set_mark(self):
            self.mark = self.off

        def reset(self):
            self.off = self.mark

    A = Arena()

    def pe(fn, r, w):
        return P.add("pe", fn, r, w)

    def act(fn, r, w):
        return P.add("act", fn, r, w)

    def dve(fn, r, w):
        return P.add("dve", fn, r, w)

    def pool(fn, r, w):
        return P.add("pool", fn, r, w)

    def dma(out, in_, r, w, slot, q="sp"):
        return P.add(q, lambda e: e.dma_start(out=out, in_=in_), r, w, slot=slot)

    def mm(out, lhsT, rhs, start, stop, r, w):
        return pe(lambda e: e.matmul(out, lhsT, rhs, start=start, stop=stop), r, w)

    def warm(bk, n=40, extra_reads=()):
        for _ in range(n):
            mm(bank(bk), ones_b, msb_b[:, 0:512], True, True, ["ones_b", "msb_b"] + list(extra_reads), [("pb", bk)])

    ident_f = A.alloc(128)
    ropef = A.alloc(3)
    ident_b = A.alloc(128, BF16)
    tri_b = A.alloc(128, BF16)
    ones_b = A.alloc(128, BF16)
    msb_b = A.alloc(2048, BF16)
    mda_b = A.alloc(2048, BF16)
    tri2 = A.alloc(2, BF16)
    ones2 = A.alloc(128, BF16, parts=2)
    gcols = A.alloc(24 * L)
    gsub = A.alloc(L)
    lamt = A.alloc(256 * L)
    lamw = A.alloc(8)
    neglam = A.alloc(L)
    gfin_bc = A.alloc(D)
    brt = A.alloc(36 * L)
    Gt = A.alloc(NT * 32)
    carry = [[A.alloc(512, BF16) for _ in range(2)] for _ in range(2)]
    negones_b = A.alloc(128, BF16)
    hi2 = [A.alloc(512, BF16, parts=2) for _ in range(2)]
    smalls = A.alloc(64)
    A.set_mark()
    cst_f = A.alloc(C_TOTAL)
    negsel = ropef[0:2, 2:3]

    dma(cst_f, consts_in[:, :], [], ["cst_f"], "c0")
    for l in range(L):
        dma(gcols[:, 24 * l:24 * l + 24], gcols_in[l], [], [("gcols", l)], f"c1{l}")
        dma(gsub[:, l:l + 1], gsub_in[l], [], [("gsub", l)], f"c2{l}")
        dma(lamt[:, 256 * l:256 * l + 256], lam_in[l].partition_broadcast(128), [], [("lamt", l)], f"c3{l}")
        dma(brt[:, 36 * l:36 * l + 36], b_r_in[l].partition_broadcast(128), [], [("brt", l)], f"c4{l}")
    dma(gfin_bc, gfin_in.partition_broadcast(128), [], ["gfin"], "c5")
    dve(lambda e: e.tensor_copy(out=ident_f, in_=cst_f[:, C_IDENT:C_IDENT + 128]), ["cst_f"], ["ident_f"])
    dve(lambda e: e.tensor_copy(out=ropef, in_=cst_f[:, C_ROPE:C_ROPE + 3]), ["cst_f"], ["ropef"])
    dve(lambda e: e.tensor_copy(out=ident_b, in_=cst_f[:, C_IDENT:C_IDENT + 128]), ["cst_f"], ["ident_b"])
    dve(lambda e: e.tensor_copy(out=tri_b, in_=cst_f[:, C_TRI:C_TRI + 128]), ["cst_f"], ["tri_b"])
    dve(lambda e: e.tensor_copy(out=ones_b, in_=cst_f[:, C_ONES:C_ONES + 128]), ["cst_f"], ["ones_b"])
    dve(lambda e: e.tensor_copy(out=msb_b, in_=cst_f[:, C_MSB:C_MSB + 2048]), ["cst_f"], ["msb_b"])
    dve(lambda e: e.tensor_copy(out=mda_b, in_=cst_f[:, C_MDA:C_MDA + 2048]), ["cst_f"], ["mda_b"])
    dve(lambda e: e.tensor_scalar(out=tri2, in0=cst_f[:, C_ONES:C_ONES + 2], scalar1=-1.0, scalar2=None, op0=ALU.mult), ["cst_f"], ["tri2"])
    dve(lambda e: e.tensor_copy(out=ones2, in_=cst_f[0:2, C_ONES:C_ONES + 128]), ["cst_f"], ["ones2"])
    dve(lambda e: e.tensor_scalar(out=negones_b, in0=cst_f[:, C_ONES:C_ONES + 128], scalar1=-1.0, scalar2=None, op0=ALU.mult), ["cst_f"], ["negones_b"])
    for ca in range(2):
        for cb_ in range(2):
            dve(lambda e, ca=ca, cb_=cb_: e.tensor_scalar(out=carry[ca][cb_], in0=cst_f[:, C_MSB:C_MSB + 512], scalar1=0.0, scalar2=None, op0=ALU.mult),
                ["cst_f"], [("carry", ca, cb_)])
    for l in range(L):
        lt = lamt[:, 256 * l:256 * l + 256]
        junk = smalls[:, 0:64]
        dve(lambda e, lt=lt: e.tensor_tensor(out=junk, in0=lt[:, 0:64], in1=lt[:, 64:128], op=ALU.mult), [("lamt", l)], ["junk_s"])
        dve(lambda e: e.reduce_sum(out=lamw[:, 0:1], in_=junk, axis=mybir.AxisListType.X), ["junk_s"], ["lamw"])
        dve(lambda e, lt=lt: e.tensor_tensor(out=junk, in0=lt[:, 128:192], in1=lt[:, 192:256], op=ALU.mult), [("lamt", l)], ["junk_s"])
        dve(lambda e: e.reduce_sum(out=lamw[:, 1:2], in_=junk, axis=mybir.AxisListType.X), ["junk_s"], ["lamw"])
        act(lambda e: e.activation(out=lamw[:, 2:4], in_=lamw[:, 0:2], func=AF.Exp), ["lamw"], ["lamw2"])
        dve(lambda e: e.tensor_tensor(out=lamw[:, 4:5], in0=lamw[:, 3:4], in1=lamw[:, 2:3], op=ALU.subtract), ["lamw2"], ["lamw3"])
        dve(lambda e, l=l: e.tensor_scalar(out=neglam[:, l:l + 1], in0=lamw[:, 4:5], scalar1=-float(lam_inits[l]), scalar2=None, op0=ALU.add),
            ["lamw3"], [("neglam", l)])

    def phase_rope():
        A.reset()
        posi = A.alloc(S, I32)
        ang = A.alloc(S)
        tq = A.alloc(S)
        ki = A.alloc(S, I32)
        rr = A.alloc(S)
        dma(posi, pos_in.partition_broadcast(128), [], ["posi"], "c0")
        dve(lambda e: e.tensor_copy(out=ang, in_=posi), ["posi"], ["ang"])
        dve(lambda e: e.tensor_scalar(out=ang, in0=ang, scalar1=ropef[:, 0:1], scalar2=None, op0=ALU.mult), ["ang", "ropef"], ["ang"])
        for which in range(2):
            shift = math.pi / 2 if which == 0 else 0.0
            dve(lambda e, shift=shift: e.tensor_scalar(out=tq, in0=ang, scalar1=shift, scalar2=1.0 / TWO_PI, op0=ALU.add, op1=ALU.mult), ["ang"], ["tq"])
            dve(lambda e: e.tensor_copy(out=ki, in_=tq), ["tq"], ["ki"])
            dve(lambda e: e.tensor_copy(out=tq, in_=ki), ["ki"], ["tq"])
            dve(lambda e: e.scalar_tensor_tensor(out=rr, in0=tq, scalar=-TWO_PI, in1=ang, op0=ALU.mult, op1=ALU.add), ["tq", "ang"], ["rr"])
            if which == 0:
                dve(lambda e: e.tensor_scalar(out=rr, in0=rr, scalar1=math.pi / 2, scalar2=None, op0=ALU.add), ["rr"], ["rr"])
            dve(lambda e: e.tensor_scalar(out=rr, in0=rr, scalar1=math.pi, scalar2=-math.pi, op0=ALU.min, op1=ALU.max), ["rr"], ["rr"])
            if which == 0:
                act(lambda e: e.activation(out=tq, in_=rr, func=AF.Sin), ["rr"], ["tq"])
            else:
                act(lambda e: e.activation(out=tq, in_=rr, func=AF.Sin), ["rr"], ["tq"])
                dve(lambda e: e.tensor_scalar(out=tq, in0=tq, scalar1=ropef[:, 1:2], scalar2=None, op0=ALU.mult), ["tq", "ropef"], ["tq"])
            dma(ropeT[which], tq, ["tq"], [("ropeT", which)], "st0")
        P.barrier()

    def load_cast(dst_b, src_dram, nchunk, ncol, stage, skeys, dkey, slot, gcol=None, gkey=None, eng="pool"):
        st3 = stage.rearrange("p (c n) -> p c n", c=nchunk)
        dma(st3, src_dram.rearrange("(c p) n -> p c n", p=128), [], list(skeys), slot)
        add = pool if eng == "pool" else (dve if eng == "dve" else act)
        if gcol is None:
            add(lambda e: e.tensor_copy(out=dst_b, in_=st3), list(skeys), [dkey])
        else:
            add(lambda e: e.tensor_tensor(out=dst_b, in0=st3, in1=gcol.unsqueeze(2).broadcast_to([128, nchunk, ncol]), op=ALU.mult),
                list(skeys) + [gkey], [dkey])

    def rms_rstd(xt, ss, rstd, junk, keyx, keyr):
        act(lambda e: e.activation(out=junk, in_=xt, func=AF.Square, accum_out=ss), [keyx], ["junk_n", keyr + "_ss"])
        dve(lambda e: e.tensor_scalar(out=ss, in0=ss, scalar1=1.0 / D, scalar2=EPS, op0=ALU.mult, op1=ALU.add), [keyr + "_ss"], [keyr + "_ss2"])
        act(lambda e: e.activation(out=ss, in_=ss, func=AF.Ln), [keyr + "_ss2"], [keyr + "_ss3"])
        act(lambda e: e.activation(out=rstd, in_=ss, func=AF.Exp, scale=-0.5), [keyr + "_ss3"], [keyr])

    def phase_inproj(l):
        A.reset()
        xsrc = x_in if l == 0 else xs
        GP = 256
        NGP = S // GP
        TPG = GP // 128
        Wb = A.alloc(8 * 6144, BF16).rearrange("p (c n) -> p c n", c=8)
        stage = [A.alloc(8 * 512) for _ in range(2)]
        xt = [A.alloc(D) for _ in range(2 * TPG)]
        hb = [A.alloc(D, BF16) for _ in range(2)]
        hT = [A.alloc(8 * GP, BF16).rearrange("p (c n) -> p c n", c=8) for _ in range(2)]
        vst = [A.alloc(TPG * 1024, BF16).rearrange("p (t n) -> p t n", t=TPG) for _ in range(2)]
        rope = [A.alloc(2 * GP).rearrange("p (w n) -> p w n", w=2) for _ in range(2)]
        t1 = A.alloc(GP)
        t2 = A.alloc(GP)
        junk = A.alloc(D)
        ss = A.alloc(4)
        gm = gcols[:, 24 * l:24 * l + 8]
        for cb in range(12):
            st = stage[cb % 2]
            st3 = st.rearrange("p (c n) -> p c n", c=8)
            dma(st3, w_in_in[l][:, cb * 512:(cb + 1) * 512].rearrange("(c p) n -> p c n", p=128), [], [("stage", cb % 2)], f"stg{cb % 2}")
            eng = pool if cb % 2 == 0 else dve
            eng(lambda e, st3=st3, cb=cb: e.tensor_tensor(out=Wb[:, :, cb * 512:(cb + 1) * 512], in0=st3,
                                                           in1=gm.unsqueeze(2).broadcast_to([128, 8, 512]), op=ALU.mult),
                [("stage", cb % 2), ("gcols", l)], [("Wb", cb)])
        P.barrier()
        fm = [stage[i].bitcast(BF16).rearrange("p (c n) -> p c n", n=GP)[:, 0:32, :] for i in range(2)]
        assert 32 * GP * 2 <= 8 * 512 * 4

        def load_x(g):
            for t in range(TPG):
                tt = g * TPG + t
                b = tt % (2 * TPG)
                dma(xt[b], xsrc[tt * 128:(tt + 1) * 128, :], [("xs", tt)], [("xt", b)], f"xt{b}")
            dma(rope[g % 2], ropeT[:, :, g * GP:(g + 1) * GP].rearrange("w p n -> p w n"), [("ropeT", 0), ("ropeT", 1)], [("rope", g % 2)], f"rope{g % 2}")

        load_x(0)
        COL = dict(sbq=0, sbk=512, sbv=1024, daq=1536, dak=2048, dav=2560, ga=3072, gb=4096, daqp=5120, dakp=5632)
        pb = [0]

        def nb():
            pb[0] = (pb[0] + 1) % 6
            return pb[0]

        for g in range(NGP):
            if g + 1 < NGP:
                load_x(g + 1)
            hTg = hT[g % 2]
            for t in range(TPG):
                tt = g * TPG + t
                b = tt % (2 * TPG)
                x_t = xt[b]
                h_t = hb[tt % 2]
                rstd = ss[:, 1:2]
                rms_rstd(x_t, ss[:, 0:1], rstd, junk, ("xt", b), "rstd1")
                dve(lambda e, x_t=x_t, h_t=h_t: e.tensor_scalar(out=h_t, in0=x_t, scalar1=rstd, scalar2=None, op0=ALU.mult),
                    [("xt", b), "rstd1"], [("hb", tt % 2)])
                tb = bank(6 + tt % 2).bitcast(BF16)
                for c in range(8):
                    pe(lambda e, c=c, tb=tb, h_t=h_t: e.transpose(tb[:, c * 128:(c + 1) * 128], h_t[:, c * 128:(c + 1) * 128], ident_b),
                       [("hb", tt % 2), "ident_b"], [("pb", 6 + tt % 2)])
                act(lambda e, tb=tb, hTg=hTg, t=t: e.activation(out=hTg[:, :, t * 128:(t + 1) * 128], in_=tb.rearrange("p (c n) -> p c n", c=8), func=AF.Copy),
                    [("pb", 6 + tt % 2)], [("hT", g % 2, t)])
            hkeys = [("hT", g % 2, t) for t in range(TPG)]
            fmg = fm[g % 2]

            def proj(col, bk):
                for c in range(8):
                    mm(bank(bk)[:, 0:GP], Wb[:, c, col:col + 128], hTg[:, c, :], c == 0, c == 7,
                       hkeys + [("Wb", col // 512)], [("pb", bk)])

            for k in range(32):
                if k < 4:
                    col, kind = COL["sbq"] + k * 128, "q"
                elif k < 8:
                    col, kind = COL["sbk"] + (k - 4) * 128, "k"
                elif k < 12:
                    col, kind = COL["daq"] + (k - 8) * 128, "rq"
                elif k < 16:
                    col, kind = COL["dak"] + (k - 12) * 128, "rk"
                elif k < 24:
                    col, kind = COL["ga"] + (k - 16) * 128, "s"
                else:
                    col, kind = COL["gb"] + (k - 24) * 128, "s"
                bk = nb()
                proj(col, bk)
                dst = fmg[:, k, :]
                fmk = ("fm", g % 2, k)
                src = bank(bk)[:, 0:GP]
                if kind == "q":
                    act(lambda e, dst=dst, src=src: e.activation(out=dst, in_=src, func=AF.Copy, scale=0.125), [("pb", bk)], [fmk])
                elif kind == "k":
                    act(lambda e, dst=dst, src=src: e.activation(out=dst, in_=src, func=AF.Copy), [("pb", bk)], [fmk])
                elif kind == "s":
                    act(lambda e, dst=dst, src=src: e.activation(out=dst, in_=src, func=AF.Sigmoid), [("pb", bk)], [fmk])
                else:
                    pcol = (COL["daqp"] if kind == "rq" else COL["dakp"]) + (col % 512)
                    bk2 = nb()
                    proj(pcol, bk2)
                    src2 = bank(bk2)[:, 0:GP]
                    rp = rope[g % 2]
                    sc = 0.125 if kind == "rq" else 1.0
                    dve(lambda e, src=src, rp=rp, sc=sc: e.scalar_tensor_tensor(out=t1, in0=src, scalar=sc, in1=rp[:, 0, :], op0=ALU.mult, op1=ALU.mult),
                        [("pb", bk), ("rope", g % 2)], ["t1"])
                    dve(lambda e, src2=src2, rp=rp, sc=sc: e.scalar_tensor_tensor(out=t2, in0=src2, scalar=sc, in1=rp[:, 1, :], op0=ALU.mult, op1=ALU.mult),
                        [("pb", bk2), ("rope", g % 2)], ["t2"])
                    pool(lambda e, dst=dst: e.tensor_tensor(out=dst, in0=t1, in1=t2, op=ALU.add), ["t1", "t2"], [fmk])
            vg = vst[g % 2]
            for t in range(TPG):
                for hv, col in enumerate((COL["sbv"], COL["dav"])):
                    bk = nb()
                    for c in range(8):
                        mm(bank(bk), hTg[:, c, t * 128:(t + 1) * 128], Wb[:, c, col:col + 512], c == 0, c == 7,
                           hkeys + [("Wb", col // 512)], [("pb", bk)])
                    dve(lambda e, vg=vg, t=t, hv=hv, bk=bk: e.tensor_copy(out=vg[:, t, hv * 512:(hv + 1) * 512], in_=bank(bk)), [("pb", bk)], [("vst", g % 2)])
            gs = slice(g * GP, (g + 1) * GP)
            dma(qkT[:, :, gs].rearrange("k p n -> p k n"), fmg[:, 0:16, :], [("fm", g % 2, k) for k in range(16)], [("qkT", g)], f"stg{g % 2}", q="pool")
            dma(sgT[:, :, gs].rearrange("k p n -> p k n"), fmg[:, 16:32, :], [("fm", g % 2, k) for k in range(16, 32)], [("sgT", g)], f"stgb{g % 2}", q="pool")
            dma(vsc[g * GP:(g + 1) * GP, :].rearrange("(t p) n -> p t n", p=128), vg, [("vst", g % 2)], [("vsc", g)], f"vst{g % 2}", q="pool")
        P.barrier()

    def phase_sb(l):
        A.reset()
        Vall = A.alloc(NT * 512, BF16).rearrange("p (k n) -> p k n", n=512)
        KF = A.alloc(S, BF16)
        QT = [A.alloc(S, BF16) for _ in range(2)]
        for j0 in range(0, S, 2048):
            jw = min(2048, S - j0)
            dve(lambda e, j0=j0, jw=jw: e.tensor_scalar(out=QT[0][64:128, j0:j0 + jw], in0=msb_b[64:128, 0:jw], scalar1=0.0, scalar2=None, op0=ALU.mult), ["msb_b"], [("QTz", 0)])
            dve(lambda e, j0=j0, jw=jw: e.tensor_scalar(out=QT[1][0:64, j0:j0 + jw], in0=msb_b[0:64, 0:jw], scalar1=0.0, scalar2=None, op0=ALU.mult), ["msb_b"], [("QTz", 1)])
        Eb = [A.alloc(512) for _ in range(2)]
        Ln_ = [A.alloc(512, BF16) for _ in range(3)]
        Wt = [A.alloc(512, BF16) for _ in range(3)]
        ost = [A.alloc(512, BF16) for _ in range(2)]
        nvd = 4
        for i in range(nvd):
            k0, k1 = NT * i // nvd, NT * (i + 1) // nvd
            dma(Vall[:, k0:k1, :], vsc[k0 * 128:k1 * 128, 0:512].rearrange("(k p) n -> p k n", p=128),
                [("vsc", g) for g in range(S // 256)], [("Vall", i)], f"va{i}")
        vkeys = [("Vall", i) for i in range(nvd)]

        def load_head(h):
            b = h % 2
            r0 = (h % 2) * 64
            allq = [("qkT", g) for g in range(S // 256)]
            if b == 0:
                dma(KF, qkT[4 + h // 2], allq, [("KT", 0), ("KT", 1)], "kt0")
            dma(QT[b][r0:r0 + 64, :], qkT[h // 2, r0:r0 + 64, :], allq, [("QT", b)], f"qt{b}")

        tiles = []
        chain_of = {}
        pos_of = {}
        ci = 0
        for hp in range(4):
            for qt in range(NG):
                kbs = list(range(4 * qt + 3, -1, -1))
                for i, kb in enumerate(kbs):
                    for sub in range(2):
                        h = 2 * hp + sub
                        chain_of[len(tiles)] = ci + sub
                        pos_of[len(tiles)] = i
                        tiles.append((h, qt, kb, i == 0, i == len(kbs) - 1, kb - 4 * qt if kb >= 4 * qt else -1))
                ci += 2
        n = len(tiles)

        load_head(0)
        load_head(1)

        NDUM = int(os.environ.get("KDUM", "0"))

        def zb(i):
            return i % 4

        def stepA(i):
            h, qt, kb, first, last, dj = tiles[i]
            if first and qt == 0 and h >= 2:
                load_head(h)
            b = h % 2
            if first and qt == 0 and h % 2 == 1:
                warm(zb(i), 40, [("KT", 0), ("QT", 0), ("KT", 1), ("QT", 1)])
            mm(bank(zb(i)), KF[:, kb * 128:(kb + 1) * 128], QT[b][:, qt * 512:(qt + 1) * 512], True, False,
               [("KT", b), ("QT", b), ("QTz", b)], [("pb", zb(i))])
            e_ = Eb[i % 2]
            act(lambda e, e_=e_, i=i: e.activation(out=e_, in_=bank(zb(i)), func=AF.Exp), [("pb", zb(i))], [("Eb", i % 2)])

        def stepC(i):
            h, qt, kb, first, last, dj = tiles[i]
            e_ = Eb[i % 2]
            ln = Ln_[i % 3]
            act(lambda e, e_=e_, ln=ln: e.activation(out=ln, in_=e_, func=AF.Ln, bias=1.0), [("Eb", i % 2)], [("Ln", i % 3)])
            if dj >= 0:
                dve(lambda e, ln=ln, dj=dj: e.tensor_tensor(out=ln, in0=ln, in1=msb_b[:, dj * 512:(dj + 1) * 512], op=ALU.mult),
                    [("Ln", i % 3), "msb_b"], [("Ln", i % 3)])

        def stepD(i):
            h, qt, kb, first, last, dj = tiles[i]
            c = chain_of[i]
            ln = Ln_[i % 3]
            mm(bank(zb(i)), tri_b, ln, False, first, [("Ln", i % 3), "tri_b"], [("pb", zb(i))])
            pos = pos_of[i]
            if not first:
                cr = carry[c % 2][(pos - 1) % 2]
                mm(bank(zb(i)), ones_b, cr, False, True, [("carry", c % 2, (pos - 1) % 2), "ones_b"], [("pb", zb(i))])
            if not last:
                csb = bank(6 + c % 2)[0:2, :]
                mm(bank(6 + c % 2), negones_b, ln, first, False, [("Ln", i % 3), "negones_b"], [("cs", c % 2)])
                cr = carry[c % 2][pos % 2]
                h2_ = hi2[c % 2]
                dve(lambda e, csb=csb, h2_=h2_: e.tensor_copy(out=h2_, in_=csb), [("cs", c % 2)], [("hi2", c % 2)])
                dve(lambda e, cr=cr, csb=csb, h2_=h2_: e.scalar_tensor_tensor(out=cr[0:2, :], in0=h2_, scalar=negsel, in1=csb, op0=ALU.mult, op1=ALU.add),
                    [("cs", c % 2), ("hi2", c % 2), "ropef"], [("carry", c % 2, pos % 2)])
            w = Wt[i % 3]
            act(lambda e, w=w, i=i: e.activation(out=w, in_=bank(zb(i)), func=AF.Exp), [("pb", zb(i))], [("Wt", i % 3)])
            if dj >= 0:
                dve(lambda e, w=w, dj=dj: e.tensor_tensor(out=w, in0=w, in1=msb_b[:, dj * 512:(dj + 1) * 512], op=ALU.mult),
                    [("Wt", i % 3), "msb_b"], [("Wt", i % 3)])

        def stepG(i):
            h, qt, kb, first, last, dj = tiles[i]
            c = chain_of[i]
            r0 = (h % 2) * 64
            obf = bank(4 + c % 2)
            w = Wt[i % 3]
            mm(obf, Vall[:, kb, (h // 2) * 128:(h // 2) * 128 + 128], w, first, last, [("Wt", i % 3)] + vkeys, [("ob", c % 2)])
            if last:
                o_ = ost[c % 2]
                dve(lambda e, o_=o_, obf=obf, r0=r0: e.tensor_copy(out=o_[r0:r0 + 64, :], in_=obf[r0:r0 + 64, :]), [("ob", c % 2)], [("ost", c % 2)])
                dma(atT[h // 2, r0:r0 + 64, qt * 512:(qt + 1) * 512], o_[r0:r0 + 64, :], [("ost", c % 2)], [("atT", 0, h, qt)], f"ost{c % 2}", q="pool")

        stepA(0)
        for s in range(n + 2):
            if s + 1 < n:
                stepA(s + 1)
            if s < n:
                stepC(s)
            if 0 <= s - 1 < n:
                stepD(s - 1)
            if 0 <= s - 2 < n:
                stepG(s - 2)
            if NDUM:
                warm(3, NDUM)
        P.barrier()

    def phase_da(l):
        A.reset()
        Vall = A.alloc(NT * 512, BF16).rearrange("p (k n) -> p k n", n=512)
        KT = A.alloc(S, BF16)
        QT = A.alloc(S, BF16)
        Pb = [A.alloc(1024, BF16) for _ in range(3)]
        r1 = A.alloc(512)
        r2 = A.alloc(512)
        o1 = A.alloc(512)
        o2 = A.alloc(512)
        ysq = A.alloc(512, BF16)
        yst = [A.alloc(512, BF16) for _ in range(2)]
        gcomb = A.alloc(1)
        dve(lambda e: e.tensor_scalar(out=gcomb, in0=gsub[:, l:l + 1], scalar1=float(1.0 - lam_inits[l]), scalar2=None, op0=ALU.mult),
            [("gsub", l)], ["gcomb"])
        nvd = 4
        for i in range(nvd):
            k0, k1 = NT * i // nvd, NT * (i + 1) // nvd
            dma(Vall[:, k0:k1, :], vsc[k0 * 128:k1 * 128, 512:1024].rearrange("(k p) n -> p k n", p=128),
                [("vsc", g) for g in range(S // 256)], [("Vall", i)], f"va{i}")
        vkeys = [("Vall", i) for i in range(nvd)]
        allq = [("qkT", g) for g in range(S // 256)]
        cnt = 0
        cq = 0
        for h in range(4):
            dma(KT, qkT[12 + h], allq, ["KTd"], "kt0")
            dma(QT, qkT[8 + h], allq, ["QTd"], "qt0")
            warm(0, 40, ["KTd", "QTd"] + vkeys)
            for qt in range(NG):
                nkb = 4 * qt + 4
                qs = slice(qt * 512, (qt + 1) * 512)

                def front(kb, cnt):
                    sb2 = (cnt % 2) * 2
                    ks = slice(kb * 128, (kb + 1) * 128)
                    mm(bank(sb2), KT[0:64, ks], QT[0:64, qs], True, True, ["KTd", "QTd"], [("pb", sb2)])
                    mm(bank(sb2 + 1), KT[64:128, ks], QT[64:128, qs], True, True, ["KTd", "QTd"], [("pb", sb2)])
                    pbuf = Pb[cnt % 3]
                    act(lambda e, pbuf=pbuf, sb2=sb2: e.activation(out=pbuf, in_=bank(sb2, 2), func=AF.Exp), [("pb", sb2)], [("Pb", cnt % 3)])
                    dj = kb - 4 * qt
                    if dj >= 0:
                        for m_ in range(2):
                            dve(lambda e, pbuf=pbuf, dj=dj, m_=m_: e.tensor_tensor(out=pbuf[:, m_ * 512:(m_ + 1) * 512], in0=pbuf[:, m_ * 512:(m_ + 1) * 512],
                                                                                    in1=mda_b[:, dj * 512:(dj + 1) * 512], op=ALU.mult),
                                [("Pb", cnt % 3), "mda_b"], [("Pb", cnt % 3)])

                def back(kb, cnt):
                    pbuf = Pb[cnt % 3]
                    first, last = kb == 0, kb == nkb - 1
                    vv = Vall[:, kb, h * 128:(h + 1) * 128]
                    rk = [("Pb", cnt % 3)] + vkeys
                    mm(bank(4), vv, pbuf[:, 0:512], first, last, rk, [("pb", 4)])
                    mm(bank(5), vv, pbuf[:, 512:1024], first, last, rk, [("pb", 5)])
                    mm(bank(6), ones_b, pbuf[:, 0:512], first, last, rk + ["ones_b"], [("pb", 6)])
                    mm(bank(7), ones_b, pbuf[:, 512:1024], first, last, rk + ["ones_b"], [("pb", 7)])

                import os
                kda = int(os.environ.get("KDA", "3"))
                front(0, cnt)
                for kb in range(nkb):
                    if kb + 1 < nkb:
                        front(kb + 1, cnt + kb + 1)
                    if kda >= 2:
                        back(kb, cnt + kb)
                cnt += nkb
                if kda < 3:
                    continue
                if kda == 10:
                    dve(lambda e: e.reciprocal(out=r1, in_=bank(6)), [("pb", 6)], ["r1"])
                    continue
                if kda == 11:
                    dve(lambda e: e.tensor_copy(out=r1, in_=bank(6)), [("pb", 6)], ["r1"])
                    dve(lambda e: e.reciprocal(out=r1, in_=r1), ["r1"], ["r1"])
                    dve(lambda e: e.tensor_tensor(out=o1, in0=bank(4), in1=r1, op=ALU.mult), [("pb", 4), "r1"], ["o1"])
                    continue
                act(lambda e: e.activation(out=r1, in_=bank(6), func=AF.Ln), [("pb", 6)], ["r1"])
                act(lambda e: e.activation(out=r1, in_=r1, func=AF.Exp, scale=-1.0), ["r1"], ["r1"])
                act(lambda e: e.activation(out=r2, in_=bank(7), func=AF.Ln), [("pb", 7)], ["r2"])
                act(lambda e: e.activation(out=r2, in_=r2, func=AF.Exp, scale=-1.0), ["r2"], ["r2"])
                dve(lambda e: e.tensor_tensor(out=o1, in0=bank(4), in1=r1, op=ALU.mult), [("pb", 4), "r1"], ["o1"])
                dve(lambda e: e.tensor_tensor(out=o2, in0=bank(5), in1=r2, op=ALU.mult), [("pb", 5), "r2"], ["o2"])
                dve(lambda e: e.scalar_tensor_tensor(out=o1, in0=o2, scalar=neglam[:, l:l + 1], in1=o1, op0=ALU.mult, op1=ALU.add),
                    ["o1", "o2", ("neglam", l)], ["o1"])
                if kda == 12:
                    continue
                dve(lambda e: e.tensor_tensor(out=ysq, in0=o1, in1=o1, op=ALU.mult), ["o1"], ["ysq"])
                if kda == 13:
                    continue
                mm(bank(6), ones_b, ysq, True, True, ["ysq", "ones_b"], [("pb", 6)])
                dve(lambda e: e.tensor_scalar(out=r1, in0=bank(6), scalar1=1.0 / 128, scalar2=EPS, op0=ALU.mult, op1=ALU.add), [("pb", 6)], ["r1"])
                if kda == 14:
                    continue
                act(lambda e: e.activation(out=r1, in_=r1, func=AF.Ln), ["r1"], ["r1"])
                act(lambda e: e.activation(out=r1, in_=r1, func=AF.Exp, scale=-0.5), ["r1"], ["r1"])
                dve(lambda e: e.tensor_tensor(out=o1, in0=o1, in1=r1, op=ALU.mult), ["o1", "r1"], ["o1"])
                y_ = yst[cq % 2]
                dve(lambda e, y_=y_: e.tensor_scalar(out=y_, in0=o1, scalar1=gcomb, scalar2=None, op0=ALU.mult), ["o1", "gcomb"], [("yst", cq % 2)])
                dma(atT[4 + h, :, qs], y_, [("yst", cq % 2)], [("atT", 1, h, qt)], f"yst{cq % 2}", q="pool")
                cq += 1
        P.barrier()

    def phase_merge(l):
        A.reset()
        xsrc = x_in if l == 0 else xs
        Wa = A.alloc(4 * D, BF16).rearrange("p (c n) -> p c n", c=4)
        Wbb = A.alloc(4 * D, BF16).rearrange("p (c n) -> p c n", c=4)
        Wo = A.alloc(8 * D, BF16).rearrange("p (c n) -> p c n", c=8)
        Wr = A.alloc(8 * 36).rearrange("p (c n) -> p c n", c=8)
        stage = A.alloc(4 * D)
        load_cast(Wa, w_a_in[l], 4, D, stage, ["stg4"], "Wa", "stg0", eng="pool")
        load_cast(Wbb, w_b_in[l], 4, D, stage, ["stg4"], "Wbb", "stg0", eng="pool")
        load_cast(Wo[:, 0:4, :], w_o_in[l][0:512, :], 4, D, stage, ["stg4"], "Wo", "stg0", eng="pool")
        load_cast(Wo[:, 4:8, :], w_o_in[l][512:1024, :], 4, D, stage, ["stg4"], "Wo2", "stg0", eng="pool")
        dma(Wr, w_r_in[l].rearrange("(c p) n -> p c n", p=128), [], ["Wr0"], "c0")
        gf = gcols[:, 24 * l + 8:24 * l + 16]
        dve(lambda e: e.tensor_tensor(out=Wr, in0=Wr, in1=gf.unsqueeze(2).broadcast_to([128, 8, 36]), op=ALU.mult), ["Wr0", ("gcols", l)], ["Wr"])
        aT = [A.alloc(4 * 512, BF16).rearrange("p (c n) -> p c n", c=4) for _ in range(2)]
        bT = [A.alloc(4 * 512, BF16).rearrange("p (c n) -> p c n", c=4) for _ in range(2)]
        sA = [A.alloc(16 * 512, BF16).rearrange("p (c n) -> p c n", c=16) for _ in range(2)]
        mT = A.alloc(8 * 512, BF16).rearrange("p (c n) -> p c n", c=8)
        t1 = A.alloc(512)
        t2 = A.alloc(512)
        xt = [A.alloc(D) for _ in range(2)]
        hf = A.alloc(D)
        hbb = A.alloc(D, BF16)
        hfT = A.alloc(D)
        h2st = [A.alloc(8 * 512, BF16).rearrange("p (c n) -> p c n", c=8) for _ in range(2)]
        junk = A.alloc(D)
        ss = A.alloc(4)
        rw = A.alloc(36 * 8)
        brl = brt[:, 36 * l:36 * l + 36]

        def load_g(g):
            gs = slice(g * 512, (g + 1) * 512)
            b = g % 2
            dma(aT[b], atT[0:4, :, gs].rearrange("k p n -> p k n"), [("atT", 0, h, g) for h in range(8)], [("aT", b)], f"aT{b}")
            dma(bT[b], atT[4:8, :, gs].rearrange("k p n -> p k n"), [("atT", 1, h, g) for h in range(4)], [("bT", b)], f"bT{b}")
            dma(sA[b], sgT[:, :, gs].rearrange("k p n -> p k n"), [("sgT", 2 * g), ("sgT", 2 * g + 1)], [("sA", b)], f"sA{b}")

        load_g(0)
        for g in range(NG):
            if g + 1 < NG:
                load_g(g + 1)
            b = g % 2
            for oc in range(8):
                pa = (oc % 2) * 2
                for k in range(4):
                    mm(bank(pa), Wa[:, k, oc * 128:(oc + 1) * 128], aT[b][:, k, :], k == 0, k == 3, ["Wa", ("aT", b)], [("pb", pa)])
                for k in range(4):
                    mm(bank(pa + 1), Wbb[:, k, oc * 128:(oc + 1) * 128], bT[b][:, k, :], k == 0, k == 3, ["Wbb", ("bT", b)], [("pb", pa + 1)])
                dve(lambda e, pa=pa, oc=oc, b=b: e.tensor_tensor(out=t1, in0=bank(pa), in1=sA[b][:, oc, :], op=ALU.mult), [("pb", pa), ("sA", b)], ["t1"])
                dve(lambda e, pa=pa, oc=oc, b=b: e.tensor_tensor(out=t2, in0=bank(pa + 1), in1=sA[b][:, 8 + oc, :], op=ALU.mult), [("pb", pa + 1), ("sA", b)], ["t2"])
                pool(lambda e, oc=oc: e.tensor_tensor(out=mT[:, oc, :], in0=t1, in1=t2, op=ALU.add), ["t1", "t2"], [("mT", oc)])
            mkeys = [("mT", oc) for oc in range(8)]
            for t in range(4):
                tt = g * 4 + t
                xb_ = xt[tt % 2]
                dma(xb_, xsrc[tt * 128:(tt + 1) * 128, :], [("xs", tt)], [("xt", tt % 2)], f"xt{tt % 2}")
                for half in range(2):
                    for oc in range(8):
                        mm(bank(4 + half), mT[:, oc, t * 128:(t + 1) * 128], Wo[:, oc, half * 512:(half + 1) * 512], oc == 0, oc == 7,
                           mkeys + ["Wo", "Wo2"], [("pb", 4 + half)])
                dve(lambda e, xb_=xb_: e.tensor_tensor(out=xb_, in0=xb_, in1=bank(4, 2), op=ALU.add), [("xt", tt % 2), ("pb", 4), ("pb", 5)], [("xt", tt % 2)])
                dma(xs[tt * 128:(tt + 1) * 128, :], xb_, [("xt", tt % 2)], [("xs", tt)], f"xo{tt % 2}", q="pool")
                rstd = ss[:, 1:2]
                rms_rstd(xb_, ss[:, 0:1], rstd, junk, ("xt", tt % 2), "rstd4")
                dve(lambda e, xb_=xb_: e.tensor_scalar(out=hf, in0=xb_, scalar1=rstd, scalar2=None, op0=ALU.mult), [("xt", tt % 2), "rstd4"], ["hf"])
                pool(lambda e: e.tensor_copy(out=hbb, in_=hf), ["hf"], ["hbb"])
                tb = bank(6).bitcast(BF16)
                for c in range(8):
                    pe(lambda e, c=c, tb=tb: e.transpose(tb[:, c * 128:(c + 1) * 128], hbb[:, c * 128:(c + 1) * 128], ident_b), ["hbb", "ident_b"], [("pb", 6)])
                h2g = h2st[g % 2]
                act(lambda e, tb=tb, h2g=h2g, t=t: e.activation(out=h2g[:, :, t * 128:(t + 1) * 128], in_=tb.rearrange("p (c n) -> p c n", c=8), func=AF.Copy),
                    [("pb", 6)], [("h2st", g % 2)])
                for hh in range(2):
                    for c in range(4):
                        cc = hh * 4 + c
                        pe(lambda e, c=c, cc=cc: e.transpose(bank(7)[:, c * 128:(c + 1) * 128], hf[:, cc * 128:(cc + 1) * 128], ident_f), ["hf", "ident_f"], [("pb", 7)])
                    act(lambda e, hh=hh: e.activation(out=hfT[:, hh * 512:(hh + 1) * 512], in_=bank(7), func=AF.Copy), [("pb", 7)], [("hfT", hh)])
                lg = bank(4)[:, 0:36]
                for c in range(8):
                    mm(lg, hfT[:, c * 128:(c + 1) * 128], Wr[:, c, :], c == 0, c == 7, [("hfT", 0), ("hfT", 1), "Wr"], [("pb", 4)])
                lgs = rw[:, 0:36]
                gmax = rw[:, 36:37]
                oh = rw[:, 40:44]
                pen = rw[:, 44:48]
                eg = rw[:, 48:52]
                gsum = rw[:, 52:53]
                els = rw[:, 56:88]
                emax = rw[:, 88:89]
                negm = rw[:, 89:90]
                ee = rw[:, 96:128]
                m1 = rw[:, 128:129]
                mk1 = rw[:, 136:168]
                ee2 = rw[:, 168:200]
                m2 = rw[:, 200:201]
                mk2 = rw[:, 208:240]
                den = rw[:, 240:241]
                Gd = Gt[:, tt * 32:(tt + 1) * 32]
                R = "rw"

                def dv(fn, extra_r=(), w=(R,)):
                    dve(fn, [R] + list(extra_r), list(w))

                dve(lambda e: e.tensor_tensor(out=lgs, in0=lg, in1=brl, op=ALU.add), [("pb", 4), ("brt", l)], [R])
                dv(lambda e: e.reduce_max(out=gmax, in_=lgs[:, 0:4], axis=mybir.AxisListType.X))
                dv(lambda e: e.tensor_scalar(out=oh, in0=lgs[:, 0:4], scalar1=gmax, scalar2=None, op0=ALU.is_ge))
                dv(lambda e: e.tensor_scalar(out=pen, in0=oh, scalar1=-1.0, scalar2=1e30, op0=ALU.add, op1=ALU.mult))
                dv(lambda e: e.tensor_scalar(out=negm, in0=gmax, scalar1=-1.0, scalar2=None, op0=ALU.mult))
                act(lambda e: e.activation(out=eg, in_=lgs[:, 0:4], func=AF.Exp, bias=negm, accum_out=gsum), [R], [R])
                for gi in range(4):
                    dv(lambda e, gi=gi: e.tensor_scalar(out=els[:, gi * 8:(gi + 1) * 8], in0=lgs[:, 4 + gi * 8:4 + (gi + 1) * 8], scalar1=pen[:, gi:gi + 1], scalar2=None, op0=ALU.add))
                dv(lambda e: e.reduce_max(out=emax, in_=els, axis=mybir.AxisListType.X))
                dv(lambda e: e.tensor_scalar(out=negm, in0=emax, scalar1=-1.0, scalar2=None, op0=ALU.mult))
                act(lambda e: e.activation(out=ee, in_=els, func=AF.Exp, bias=negm), [R], [R])
                dv(lambda e: e.reduce_max(out=m1, in_=ee, axis=mybir.AxisListType.X))
                dv(lambda e: e.tensor_scalar(out=mk1, in0=ee, scalar1=m1, scalar2=None, op0=ALU.is_ge))
                dv(lambda e: e.scalar_tensor_tensor(out=ee2, in0=mk1, scalar=-2.0, in1=ee, op0=ALU.mult, op1=ALU.add))
                dv(lambda e: e.reduce_max(out=m2, in_=ee2, axis=mybir.AxisListType.X))
                dv(lambda e: e.tensor_scalar(out=mk2, in0=ee2, scalar1=m2, scalar2=None, op0=ALU.is_ge))
                dv(lambda e: e.tensor_tensor(out=den, in0=m1, in1=m2, op=ALU.add))
                dv(lambda e: e.tensor_tensor(out=den, in0=den, in1=gsum, op=ALU.mult))
                dv(lambda e: e.reciprocal(out=den, in_=den))
                dv(lambda e: e.tensor_scalar(out=mk1, in0=mk1, scalar1=m1, scalar2=den, op0=ALU.mult, op1=ALU.mult))
                dv(lambda e: e.tensor_scalar(out=mk2, in0=mk2, scalar1=m2, scalar2=den, op0=ALU.mult, op1=ALU.mult))
                dve(lambda e, Gd=Gd: e.tensor_tensor(out=Gd, in0=mk1, in1=mk2, op=ALU.add), [R], [("Gt", tt)])
            dma(h2T[:, :, g * 512:(g + 1) * 512].rearrange("k p n -> p k n"), h2st[g % 2], [("h2st", g % 2)], [("h2T", g)], f"h2st{g % 2}", q="pool")
        P.barrier()

    def phase_moe(l):
        A.reset()
        TG = min(S, 2048)
        NTG = TG // 128
        hTg = A.alloc(8 * TG, BF16).rearrange("p (c n) -> p c n", c=8)
        acc = A.alloc(NTG * D).rearrange("p (t n) -> p t n", t=NTG)
        Wgu = [A.alloc(8 * 512, BF16).rearrange("p (c n) -> p c n", c=8) for _ in range(2)]
        Wd = [A.alloc(2 * D, BF16).rearrange("p (c n) -> p c n", c=2) for _ in range(2)]
        stg = A.alloc(8 * 256)
        stu = A.alloc(8 * 256)
        std = A.alloc(2 * D)
        sl = [A.alloc(256) for _ in range(2)]
        ab = [A.alloc(256, BF16) for _ in range(2)]
        aT = [A.alloc(256, BF16) for _ in range(2)]
        gf = gcols[:, 24 * l + 8:24 * l + 16]

        def load_w(e, b):
            dma(stg.rearrange("p (c n) -> p c n", c=8), w_eg_in[l, e].rearrange("(c p) n -> p c n", p=128), [], ["stg"], "stg0")
            dma(stu.rearrange("p (c n) -> p c n", c=8), w_eu_in[l, e].rearrange("(c p) n -> p c n", p=128), [], ["stu"], "stg1")
            dma(std.rearrange("p (c n) -> p c n", c=2), w_ed_in[l, e].rearrange("(c p) n -> p c n", p=128), [], ["std"], "stg2")
            pool(lambda en: en.tensor_tensor(out=Wgu[b][:, :, 0:256], in0=stg.rearrange("p (c n) -> p c n", c=8),
                                             in1=gf.unsqueeze(2).broadcast_to([128, 8, 256]), op=ALU.mult), ["stg", ("gcols", l)], [("Wgu", b, 0)])
            pool(lambda en: en.tensor_tensor(out=Wgu[b][:, :, 256:512], in0=stu.rearrange("p (c n) -> p c n", c=8),
                                             in1=gf.unsqueeze(2).broadcast_to([128, 8, 256]), op=ALU.mult), ["stu", ("gcols", l)], [("Wgu", b, 1)])
            pool(lambda en: en.tensor_copy(out=Wd[b], in_=std.rearrange("p (c n) -> p c n", c=2)), ["std"], [("Wd", b)])

        for tg in range(S // TG):
            t0 = tg * NTG
            dma(hTg, h2T[:, :, tg * TG:(tg + 1) * TG].rearrange("k p n -> p k n"), [("h2T", g) for g in range(tg * TG // 512, (tg + 1) * TG // 512)], ["hTg"], "hTg")
            dma(acc, xs[t0 * 128:(t0 + NTG) * 128, :].rearrange("(t p) n -> p t n", p=128), [("xs", t0 + t) for t in range(NTG)],
                [("acc", t) for t in range(NTG)], "accl")
            load_w(0, 0)
            load_w(1, 1)
            items = [(e, t) for e in range(NE) for t in range(NTG)]

            def front(i):
                e, t = items[i]
                b = e % 2
                hb_ = i % 2
                for c in range(8):
                    mm(bank(hb_), hTg[:, c, t * 128:(t + 1) * 128], Wgu[b][:, c, :], c == 0, c == 7, ["hTg", ("Wgu", b, 0), ("Wgu", b, 1)], [("pb", hb_)])
                s_ = sl[i % 2]
                a_ = ab[i % 2]
                act(lambda en, s_=s_, hb_=hb_: en.activation(out=s_, in_=bank(hb_)[:, 0:256], func=AF.Silu), [("pb", hb_)], [("sl", i % 2)])
                gcolumn = Gt[:, (t0 + t) * 32 + e:(t0 + t) * 32 + e + 1]
                dve(lambda en, s_=s_, a_=a_, hb_=hb_, gcolumn=gcolumn: en.scalar_tensor_tensor(out=a_, in0=s_, scalar=gcolumn, in1=bank(hb_)[:, 256:512], op0=ALU.mult, op1=ALU.mult),
                    [("sl", i % 2), ("pb", hb_), ("Gt", t0 + t)], [("ab", i % 2)])

            def mid(i):
                e, t = items[i]
                a_ = ab[i % 2]
                tb = bank(2 + i % 2).bitcast(BF16)
                for c in range(2):
                    pe(lambda en, c=c, tb=tb, a_=a_: en.transpose(tb[:, c * 128:(c + 1) * 128], a_[:, c * 128:(c + 1) * 128], ident_b), [("ab", i % 2), "ident_b"], [("pb", 2 + i % 2)])
                at_ = aT[i % 2]
                act(lambda en, at_=at_, tb=tb: en.activation(out=at_, in_=tb[:, 0:256], func=AF.Copy), [("pb", 2 + i % 2)], [("aT", i % 2)])

            def back(i):
                e, t = items[i]
                b = e % 2
                at_ = aT[i % 2]
                ob = 4 + (i % 2) * 2
                for half in range(2):
                    for c in range(2):
                        mm(bank(ob + half), at_[:, c * 128:(c + 1) * 128], Wd[b][:, c, half * 512:(half + 1) * 512], c == 0, c == 1, [("aT", i % 2), ("Wd", b)], [("pb", ob + half)])
                dve(lambda en, t=t, ob=ob: en.tensor_tensor(out=acc[:, t, :], in0=acc[:, t, :], in1=bank(ob, 2), op=ALU.add), [("acc", t), ("pb", ob), ("pb", ob + 1)], [("acc", t)])
                if items[i][1] == NTG - 1 and items[i][0] + 2 < NE:
                    load_w(items[i][0] + 2, items[i][0] % 2)

            ni = len(items)
            front(0)
            for i in range(ni + 1):
                if i + 1 < ni:
                    front(i + 1)
                if i < ni:
                    mid(i)
                if 0 <= i - 1 < ni:
                    back(i - 1)
            dma(xs[t0 * 128:(t0 + NTG) * 128, :].rearrange("(t p) n -> p t n", p=128), acc, [("acc", t) for t in range(NTG)],
                [("xs", t0 + t) for t in range(NTG)], "accs", q="pool")
        P.barrier()

    def phase_ple(l):
        A.reset()
        last = l == L - 1
        Wpg = A.alloc(8 * D, BF16).rearrange("p (c n) -> p c n", c=8)
        Wpl = A.alloc(2 * D, BF16).rearrange("p (c n) -> p c n", c=2)
        stage = A.alloc(8 * D)
        gp = gcols[:, 24 * l + 16:24 * l + 24]
        load_cast(Wpg, w_pg_in[l], 8, D, stage, ["stg6"], "Wpg", "stg0", gcol=gp, gkey=("gcols", l), eng="pool")
        load_cast(Wpl, w_ple_in[l], 2, D, stage[:, 0:2 * D], ["stg6"], "Wpl", "stg0", eng="dve")
        xt = [A.alloc(D) for _ in range(2)]
        pt = [A.alloc(256) for _ in range(2)]
        hb = [A.alloc(D, BF16) for _ in range(2)]
        pbf = [A.alloc(256, BF16) for _ in range(2)]
        hT = [A.alloc(D, BF16) for _ in range(2)]
        pT = [A.alloc(256, BF16) for _ in range(2)]
        sg = [A.alloc(D) for _ in range(2)]
        junk = A.alloc(D)
        ss = A.alloc(16)
        ot = [A.alloc(D) for _ in range(2)]
        def partA(tt):
            b = tt % 2
            dma(xt[b], xs[tt * 128:(tt + 1) * 128, :], [("xs", tt)], [("xt", b)], f"xt{b}")
            dma(pt[b], p_in[l, tt * 128:(tt + 1) * 128, :], [], [("pt", b)], f"pt{b}")
            rstd = ss[:, 4 * b + 1:4 * b + 2]
            rms_rstd(xt[b], ss[:, 4 * b:4 * b + 1], rstd, junk, ("xt", b), "rstd6%d" % b)
            dve(lambda e, b=b, rstd=rstd: e.tensor_scalar(out=hb[b], in0=xt[b], scalar1=rstd, scalar2=None, op0=ALU.mult), [("xt", b), "rstd6%d" % b], [("hb", b)])
            pool(lambda e, b=b: e.tensor_copy(out=pbf[b], in_=pt[b]), [("pt", b)], [("pbf", b)])
            tbk = 6 if b == 0 else 4
            tb = bank(tbk).bitcast(BF16)
            for c in range(8):
                pe(lambda e, c=c, tb=tb, b=b: e.transpose(tb[:, c * 128:(c + 1) * 128], hb[b][:, c * 128:(c + 1) * 128], ident_b), [("hb", b), "ident_b"], [("pb", tbk)])
            act(lambda e, tb=tb, b=b: e.activation(out=hT[b], in_=tb, func=AF.Copy), [("pb", tbk)], [("hT", b)])
            tbk2 = 7 if b == 0 else 5
            tb2 = bank(tbk2).bitcast(BF16)
            for c in range(2):
                pe(lambda e, c=c, tb2=tb2, b=b: e.transpose(tb2[:, c * 128:(c + 1) * 128], pbf[b][:, c * 128:(c + 1) * 128], ident_b), [("pbf", b), "ident_b"], [("pb", tbk2)])
            act(lambda e, tb2=tb2, b=b: e.activation(out=pT[b], in_=tb2[:, 0:256], func=AF.Copy), [("pb", tbk2)], [("pT", b)])

        def partB(tt):
            b = tt % 2
            for half in range(2):
                for c in range(8):
                    mm(bank(half), hT[b][:, c * 128:(c + 1) * 128], Wpg[:, c, half * 512:(half + 1) * 512], c == 0, c == 7, [("hT", b), "Wpg"], [("pb", half)])
            act(lambda e, b=b: e.activation(out=sg[b], in_=bank(0, 2), func=AF.Sigmoid), [("pb", 0), ("pb", 1)], [("sg", b)])
            for half in range(2):
                for c in range(2):
                    mm(bank(2 + half), pT[b][:, c * 128:(c + 1) * 128], Wpl[:, c, half * 512:(half + 1) * 512], c == 0, c == 1, [("pT", b), "Wpl"], [("pb", 2 + half)])
            dve(lambda e, b=b: e.tensor_tensor(out=sg[b], in0=sg[b], in1=bank(2, 2), op=ALU.mult), [("sg", b), ("pb", 2), ("pb", 3)], [("sg", b)])
            o_ = ot[b]
            dve(lambda e, b=b, o_=o_: e.tensor_tensor(out=o_, in0=xt[b], in1=sg[b], op=ALU.add), [("xt", b), ("sg", b)], [("ot", b)])
            if not last:
                dma(xs[tt * 128:(tt + 1) * 128, :], o_, [("ot", b)], [("xs", tt)], f"ot{b}", q="pool")
            else:
                rstd2 = ss[:, 4 * b + 3:4 * b + 4]
                rms_rstd(o_, ss[:, 4 * b + 2:4 * b + 3], rstd2, junk, ("ot", b), "rstd7%d" % b)
                dve(lambda e, o_=o_, rstd2=rstd2: e.scalar_tensor_tensor(out=o_, in0=o_, scalar=rstd2, in1=gfin_bc, op0=ALU.mult, op1=ALU.mult), [("ot", b), "rstd7%d" % b, "gfin"], [("ot", b)])
                dma(out_d[tt * 128:(tt + 1) * 128, :], o_, [("ot", b)], [("out", tt)], f"ot{b}", q="pool")

        partA(0)
        for tt in range(NT):
            if tt + 1 < NT:
                partA(tt + 1)
            partB(tt)
        P.barrier()

    P.barrier()
    import os
    nstop = int(os.environ.get("KSTOP", "99"))
    plist = [phase_rope]
    for l in range(L):
        for ph in (phase_inproj, phase_sb, phase_da, phase_merge, phase_moe, phase_ple):
            plist.append(lambda ph=ph, l=l: ph(l))
    for ph in plist[:nstop]:
        ph()
    P.finalize()

    slots = sorted(P.slotcnt.keys())
    sems = {}
    for e in Prog.CE:
        for ph in range(P.epoch[e] + 1):
            if any(op.needs_inc and op.phase == ph for op in P.ops[e]):
                sems[(e, ph)] = es.enter_context(nc.semaphore(f"s_{e}_{ph}"))
    for s in slots:
        sems[("dma", s)] = es.enter_context(nc.semaphore(f"d_{s}"))
    block = es.enter_context(nc.Block())

    def emit(engobj, ename):
        for op in P.ops[ename]:
            for k, v in op.waits:
                engobj.wait_ge(sems[k], v)
            if op.fn is None:
                continue
            ins = op.fn(engobj)
            if op.slot is not None:
                ins.then_inc(sems[("dma", op.slot)], 16)
            elif op.needs_inc:
                ins.then_inc(sems[(ename, op.phase)], 1)

    @block.sync
    def _(e):
        emit(e, "sp")

    @block.tensor
    def _(e):
        emit(e, "pe")

    @block.scalar
    def _(e):
        emit(e, "act")

    @block.vector
    def _(e):
        emit(e, "dve")

    @block.gpsimd
    def _(e):
        emit(e, "pool")

    es.close()
    return nc, len(sems), {k: len(v) for k, v in P.ops.items()}


def _perm_cols():
    idx = np.arange(512)
    i = idx % 64
    partner = np.where(i < 8, idx + 8, np.where(i < 16, idx - 8, idx))
    return partner


def make_in_maps(inputs, S, L, n_cores):
    f = lambda a: np.ascontiguousarray(np.asarray(a))
    x = f(inputs["x"])
    B = x.shape[0]
    pc = _perm_cols()
    w_in = f(inputs["w_in"])
    w_ext = np.concatenate([w_in, w_in[:, :, 1536 + pc], w_in[:, :, 2048 + pc]], axis=2)
    gcols = np.concatenate([f(inputs[k]).reshape(L, 8, 128).transpose(0, 2, 1) for k in ("g_mix", "g_ffn", "g_ple")], axis=2)
    lam = np.concatenate([f(inputs[k]) for k in ("lam_q1", "lam_k1", "lam_q2", "lam_k2")], axis=1).reshape(L, 1, 256)
    w_r = np.concatenate([f(inputs["w_router_group"]), f(inputs["w_router_expert"])], axis=2)
    b_r = np.concatenate([f(inputs["b_router_group"]), f(inputs["b_router_expert"])], axis=1).reshape(L, 1, 36)
    shared = {
        "consts": _consts_host(S),
        "gcols": np.ascontiguousarray(gcols, dtype=np.float32),
        "w_in": np.ascontiguousarray(w_ext, dtype=np.float32),
        "lam": np.ascontiguousarray(lam, dtype=np.float32),
        "gsub": f(inputs["g_subln"]).reshape(L, 128, 1),
        "w_a": f(inputs["w_br_a"]), "w_b": f(inputs["w_br_b"]), "w_o": f(inputs["w_o"]),
        "w_r": np.ascontiguousarray(w_r, dtype=np.float32), "b_r": np.ascontiguousarray(b_r, dtype=np.float32),
        "w_eg": f(inputs["w_exp_gate"]), "w_eu": f(inputs["w_exp_up"]), "w_ed": f(inputs["w_exp_down"]),
        "w_ple": f(inputs["w_ple"]), "w_pg": f(inputs["w_ple_gate"]),
        "gfin": f(inputs["g_final"]).reshape(1, D),
    }
    p = f(inputs["p"])
    pos = f(inputs["positions"]).astype(np.int32)
    maps = []
    for c in range(n_cores):
        b = (c * B) // n_cores
        m = dict(shared)
        m["x"] = np.ascontiguousarray(x[b])
        m["p"] = np.ascontiguousarray(p[:, b])
        m["pos"] = np.ascontiguousarray(pos[b].reshape(1, S))
        maps.append(m)
    return maps


_CACHE = {}
_LAST = None


def kernel(**inputs):
    x = np.asarray(inputs["x"])
    B, S, _ = x.shape
    L = np.asarray(inputs["g_mix"]).shape[0]
    lam_inits = [0.8 - 0.6 * math.exp(-0.3 * i) for i in range(L)]
    key = (S, L)
    if key not in _CACHE:
        _CACHE[key] = build_program(S, L, lam_inits)[0]
    nc = _CACHE[key]
    maps = make_in_maps(inputs, S, L, N_CORES)
    res = run_bass_kernel_spmd(nc, maps, core_ids=list(range(N_CORES)))
    global _LAST
    _LAST = res.results
    per = N_CORES // B
    out = np.stack([np.asarray(res.results[b * per]["out"]) for b in range(B)], axis=0)
    return out.astype(np.float32)
```
